# Optimizing a Trainium2 kernel written in Bass

```python
import jax, jax.numpy as jnp
from jax import lax
import numpy as np

D_MODEL = 1024
BATCH = 4
SEQ = 8192
DEPTH = 1
DEC_BATCH = 32
DEC_SEQ = 64
PAST_LEN = 1024

CHUNK = 64
PLE_DIM = 256
N_HEADS = 8
QK_NOPE = 64
QK_ROPE = 32
QK_DIM = QK_NOPE + QK_ROPE
V_DIM = 64
Q_LORA = 256
KV_LORA = 256
ROPE_THETA = 10000.0
ATTN_SCALE = QK_DIM ** -0.5
CONV_DIM = 512
CONV_W = 3
N_GROUPS = 4
EXPERTS_PER_GROUP = 8
N_EXPERTS = N_GROUPS * EXPERTS_PER_GROUP
TOP_K = 2
EXPERT_HIDDEN = 256
MOE_BLOCK = 128
Q_BLOCK = 128
EPS = 1e-6
IN_COLS = Q_LORA + KV_LORA + QK_ROPE + 3 * CONV_DIM + 2 * D_MODEL

kernel_name = "mla_shortconv_gated_hier_moe_stream_step"


def rmsnorm(x, g):
    xf = x.astype(jnp.float32)
    y = xf * lax.rsqrt(jnp.mean(xf * xf, axis=-1, keepdims=True) + EPS)
    return (y * g.astype(jnp.float32)).astype(x.dtype)


def rope_tables(pos):
    inv = 1.0 / (ROPE_THETA ** (jnp.arange(0, QK_ROPE, 2, dtype=jnp.float32) / QK_ROPE))
    ang = pos.astype(jnp.float32)[:, None] * inv[None, :]
    return jnp.cos(ang), jnp.sin(ang)


def apply_rope(x, cos, sin):
    xf = x.astype(jnp.float32)
    x1, x2 = jnp.split(xf, 2, axis=-1)
    return jnp.concatenate([x1 * cos - x2 * sin, x1 * sin + x2 * cos], axis=-1).astype(x.dtype)


def split_cols(z):
    sizes = (Q_LORA, KV_LORA, QK_ROPE, CONV_DIM, CONV_DIM, CONV_DIM, D_MODEL, D_MODEL)
    out, off = [], 0
    for n in sizes:
        out.append(z[..., off:off + n])
        off += n
    return out


def attn_probs(s, dtype):
    return jax.nn.softmax(s.astype(jnp.float32) * ATTN_SCALE, axis=-1).astype(dtype)


def attention_prompt(q_nope, q_pe, k_nope, k_pe, v):
    B, S = q_nope.shape[0], q_nope.shape[1]
    nq = S // Q_BLOCK
    qn_b = q_nope.reshape(B, nq, Q_BLOCK, N_HEADS, QK_NOPE).transpose(1, 0, 2, 3, 4)
    qp_b = q_pe.reshape(B, nq, Q_BLOCK, N_HEADS, QK_ROPE).transpose(1, 0, 2, 3, 4)
    key_chunk = jnp.arange(S) // CHUNK

    def one_block(args):
        qn, qp, i = args
        s = (jnp.einsum('bqhd,bkhd->bhqk', qn, k_nope)
             + jnp.einsum('bqhr,bkr->bhqk', qp, k_pe)).astype(jnp.float32)
        q_chunk = (i * Q_BLOCK + jnp.arange(Q_BLOCK)) // CHUNK
        mask = key_chunk[None, :] <= q_chunk[:, None]
        s = jnp.where(mask[None, None], s, -jnp.inf)
        p = attn_probs(s, v.dtype)
        return jnp.einsum('bhqk,bkhd->bqhd', p, v)

    o = lax.map(one_block, (qn_b, qp_b, jnp.arange(nq)))
    return o.transpose(1, 0, 2, 3, 4).reshape(B, S, N_HEADS * V_DIM)


def attention_sample(q_nope, q_pe, k_nope, k_pe, v):
    B, S = q_nope.shape[0], q_nope.shape[1]
    s = (jnp.einsum('bqhd,bkhd->bhqk', q_nope, k_nope)
         + jnp.einsum('bqhr,bkr->bhqk', q_pe, k_pe))
    p = attn_probs(s, v.dtype)
    return jnp.einsum('bhqk,bkhd->bqhd', p, v).reshape(B, S, N_HEADS * V_DIM)


def hier_moe(h, w_rg, b_rg, w_re, b_re, w1, w3, w2):
    N, D = h.shape
    g_logit = (h @ w_rg).astype(jnp.float32) + b_rg.astype(jnp.float32)
    g_prob = jax.nn.softmax(g_logit, axis=-1)
    g_idx = jnp.argmax(g_logit, axis=-1).astype(jnp.int32)
    g_p = jnp.take_along_axis(g_prob, g_idx[:, None], axis=-1)[:, 0]
    e_logit = ((h @ w_re).astype(jnp.float32) + b_re.astype(jnp.float32)).reshape(N, N_GROUPS, EXPERTS_PER_GROUP)
    e_logit = jnp.take_along_axis(e_logit, g_idx[:, None, None], axis=1)[:, 0]
    e_prob = jax.nn.softmax(e_logit, axis=-1)
    top_p, top_i = lax.top_k(e_prob, TOP_K)
    gate = g_p[:, None] * top_p / jnp.sum(top_p, axis=-1, keepdims=True)
    expert = g_idx[:, None] * EXPERTS_PER_GROUP + top_i.astype(jnp.int32)

    A = N * TOP_K
    e_flat = expert.reshape(A)
    w_flat = gate.reshape(A)
    tok = jnp.arange(A, dtype=jnp.int32) // TOP_K
    order = jnp.argsort(e_flat)
    e_sorted = e_flat[order]
    counts = jnp.zeros((N_EXPERTS,), jnp.int32).at[e_flat].add(1)
    padded = (counts + MOE_BLOCK - 1) // MOE_BLOCK * MOE_BLOCK
    start = jnp.cumsum(counts) - counts
    pend = jnp.cumsum(padded)
    pstart = pend - padded
    dest = pstart[e_sorted] + jnp.arange(A, dtype=jnp.int32) - start[e_sorted]
    NB = -(-A // MOE_BLOCK) + N_EXPERTS
    P = NB * MOE_BLOCK
    buf_tok = jnp.full((P,), N, jnp.int32).at[dest].set(tok[order])
    buf_w = jnp.zeros((P,), jnp.float32).at[dest].set(w_flat[order])
    blk_e = jnp.minimum(jnp.searchsorted(pend, jnp.arange(NB, dtype=jnp.int32) * MOE_BLOCK, side='right'),
                        N_EXPERTS - 1).astype(jnp.int32)
    h_pad = jnp.concatenate([h, jnp.zeros((1, D), h.dtype)], axis=0)
    xb = h_pad[buf_tok].reshape(NB, MOE_BLOCK, D)

    def expert_block(args):
        xblk, eid = args
        a = xblk @ w1[eid]
        b = xblk @ w3[eid]
        return (jax.nn.silu(a) * b) @ w2[eid]

    yb = lax.map(expert_block, (xb, blk_e)).reshape(P, D)
    y = jax.ops.segment_sum(yb * buf_w[:, None].astype(yb.dtype), buf_tok, num_segments=N + 1)[:N]
    return y.astype(h.dtype)


def layer(x, p, past_lat, past_kpe, past_conv,
          g_mix, w_in, g_cq, w_uq, g_qn, g_qr, g_ckv, w_ukv, g_kn, g_kr, w_oa,
          conv_w, w_oc, w_o, g_ffn, w_rg, b_rg, w_re, b_re, w1, w3, w2,
          g_ple, w_pg, w_ple):
    B, S, D = x.shape
    past_len = 0 if past_lat is None else past_lat.shape[1]
    pos = past_len + jnp.arange(S)
    h = rmsnorm(x, g_mix)
    c_q, c_kv, k_r, conv_b, conv_c, conv_x, gate_a, gate_c = split_cols(h @ w_in)

    cos, sin = rope_tables(pos)
    q = (rmsnorm(c_q, g_cq) @ w_uq).reshape(B, S, N_HEADS, QK_DIM)
    q_nope = rmsnorm(q[..., :QK_NOPE], g_qn)
    q_pe = apply_rope(rmsnorm(q[..., QK_NOPE:], g_qr), cos[:, None, :], sin[:, None, :])
    lat_new = rmsnorm(c_kv, g_ckv)
    kpe_new = apply_rope(rmsnorm(k_r, g_kr), cos, sin)
    if past_lat is None:
        lat_all, kpe_all = lat_new, kpe_new
    else:
        lat_all = jnp.concatenate([past_lat, lat_new], axis=1)
        kpe_all = jnp.concatenate([past_kpe, kpe_new], axis=1)
    T = lat_all.shape[1]
    kv = (lat_all @ w_ukv).reshape(B, T, N_HEADS, QK_NOPE + V_DIM)
    k_nope = rmsnorm(kv[..., :QK_NOPE], g_kn)
    v = kv[..., QK_NOPE:]
    if past_lat is None:
        attn = attention_prompt(q_nope, q_pe, k_nope, kpe_all, v)
    else:
        attn = attention_sample(q_nope, q_pe, k_nope, kpe_all, v)
    y_a = attn @ w_oa

    u = conv_c * conv_x
    if past_conv is None:
        up = jnp.pad(u, ((0, 0), (CONV_W - 1, 0), (0, 0)))
    else:
        up = jnp.concatenate([past_conv, u], axis=1)
    cv = sum(conv_w[k] * up[:, k:k + S] for k in range(CONV_W))
    y_c = (conv_b * cv) @ w_oc
    conv_new = up[:, -(CONV_W - 1):]

    m = jax.nn.sigmoid(gate_a) * y_a + jax.nn.sigmoid(gate_c) * y_c
    x = x + m @ w_o

    h2 = rmsnorm(x, g_ffn).reshape(B * S, D)
    x = x + hier_moe(h2, w_rg, b_rg, w_re, b_re, w1, w3, w2).reshape(B, S, D)

    x = x + jax.nn.sigmoid(rmsnorm(x, g_ple) @ w_pg) * (p @ w_ple)
    return x, lat_new, kpe_new, conv_new


def _normal(key, shape, scale):
    return jax.random.normal(key, shape, jnp.float32) * scale


def _gain(key, shape):
    return 1.0 + 0.05 * jax.random.normal(key, shape, jnp.float32)


def setup_inputs(seed: int = 0) -> dict:
    key = jax.random.key(seed)
    ks = jax.random.split(key, 32)
    L = DEPTH
    return {
        "x_prompt": _normal(ks[0], (BATCH, SEQ, D_MODEL), 1.0),
        "x_sample": _normal(ks[1], (DEC_BATCH, DEC_SEQ, D_MODEL), 1.0),
        "cache_kv_latent": _normal(ks[2], (L, DEC_BATCH, PAST_LEN, KV_LORA), 1.0),
        "cache_k_rope": _normal(ks[3], (L, DEC_BATCH, PAST_LEN, QK_ROPE), 1.0),
        "state_conv": _normal(ks[4], (L, DEC_BATCH, CONV_W - 1, CONV_DIM), 1.0),
        "p_prompt": _normal(ks[5], (L, BATCH, SEQ, PLE_DIM), 1.0),
        "p_sample": _normal(ks[6], (L, DEC_BATCH, DEC_SEQ, PLE_DIM), 1.0),
        "g_mix": _gain(ks[7], (L, D_MODEL)),
        "w_in": _normal(ks[8], (L, D_MODEL, IN_COLS), D_MODEL ** -0.5),
        "g_cq": _gain(ks[9], (L, Q_LORA)),
        "w_uq": _normal(ks[10], (L, Q_LORA, N_HEADS * QK_DIM), Q_LORA ** -0.5),
        "g_qn": _gain(ks[11], (L, QK_NOPE)),
        "g_qr": _gain(ks[12], (L, QK_ROPE)),
        "g_ckv": _gain(ks[13], (L, KV_LORA)),
        "w_ukv": _normal(ks[14], (L, KV_LORA, N_HEADS * (QK_NOPE + V_DIM)), KV_LORA ** -0.5),
        "g_kn": _gain(ks[15], (L, QK_NOPE)),
        "g_kr": _gain(ks[16], (L, QK_ROPE)),
        "w_oa": _normal(ks[17], (L, N_HEADS * V_DIM, D_MODEL), (N_HEADS * V_DIM) ** -0.5),
        "conv_w": _normal(ks[18], (L, CONV_W, CONV_DIM), CONV_W ** -0.5),
        "w_oc": _normal(ks[19], (L, CONV_DIM, D_MODEL), CONV_DIM ** -0.5),
        "w_o": _normal(ks[20], (L, D_MODEL, D_MODEL), D_MODEL ** -0.5),
        "g_ffn": _gain(ks[21], (L, D_MODEL)),
        "w_rg": _normal(ks[22], (L, D_MODEL, N_GROUPS), D_MODEL ** -0.5),
        "b_rg": _normal(ks[23], (L, N_GROUPS), 0.01),
        "w_re": _normal(ks[24], (L, D_MODEL, N_EXPERTS), D_MODEL ** -0.5),
        "b_re": _normal(ks[25], (L, N_EXPERTS), 0.01),
        "w1": _normal(ks[26], (L, N_EXPERTS, D_MODEL, EXPERT_HIDDEN), D_MODEL ** -0.5),
        "w3": _normal(ks[27], (L, N_EXPERTS, D_MODEL, EXPERT_HIDDEN), D_MODEL ** -0.5),
        "w2": _normal(ks[28], (L, N_EXPERTS, EXPERT_HIDDEN, D_MODEL), EXPERT_HIDDEN ** -0.5),
        "g_ple": _gain(ks[29], (L, D_MODEL)),
        "w_pg": _normal(ks[30], (L, D_MODEL, D_MODEL), D_MODEL ** -0.5),
        "w_ple": _normal(ks[31], (L, PLE_DIM, D_MODEL), PLE_DIM ** -0.5),
    }


def reference(x_prompt, x_sample, cache_kv_latent, cache_k_rope, state_conv, p_prompt, p_sample,
              g_mix, w_in, g_cq, w_uq, g_qn, g_qr, g_ckv, w_ukv, g_kn, g_kr, w_oa,
              conv_w, w_oc, w_o, g_ffn, w_rg, b_rg, w_re, b_re, w1, w3, w2,
              g_ple, w_pg, w_ple):
    xp, xs = x_prompt, x_sample
    lat_p, kpe_p, conv_p, lat_s, kpe_s, conv_s = [], [], [], [], [], []
    for i in range(DEPTH):
        wts = (g_mix[i], w_in[i], g_cq[i], w_uq[i], g_qn[i], g_qr[i], g_ckv[i], w_ukv[i], g_kn[i], g_kr[i],
               w_oa[i], conv_w[i], w_oc[i], w_o[i], g_ffn[i], w_rg[i], b_rg[i], w_re[i], b_re[i],
               w1[i], w3[i], w2[i], g_ple[i], w_pg[i], w_ple[i])
        xp, a, b, c = layer(xp, p_prompt[i], None, None, None, *wts)
        lat_p.append(a); kpe_p.append(b); conv_p.append(c)
        xs, a, b, c = layer(xs, p_sample[i], cache_kv_latent[i], cache_k_rope[i], state_conv[i], *wts)
        lat_s.append(a); kpe_s.append(b); conv_s.append(c)
    new_kv_latent_prompt = jnp.stack(lat_p, axis=0)
    new_k_rope_prompt = jnp.stack(kpe_p, axis=0)
    new_conv_prompt = jnp.stack(conv_p, axis=0)
    new_kv_latent_sample = jnp.stack(lat_s, axis=0)
    new_k_rope_sample = jnp.stack(kpe_s, axis=0)
    new_conv_sample = jnp.stack(conv_s, axis=0)
    return (xp, xs, new_kv_latent_prompt, new_k_rope_prompt, new_conv_prompt,
            new_kv_latent_sample, new_k_rope_sample, new_conv_sample)
```

```python
import numpy as np
from contextlib import ExitStack
import concourse.bass as bass
import concourse.mybir as mybir
from concourse.bass_utils import run_bass_kernel_spmd

F32 = mybir.dt.float32
BF16 = mybir.dt.bfloat16
AF = mybir.ActivationFunctionType
ALU = mybir.AluOpType
AX = mybir.AxisListType

D = 1024
SEQ = 8192
NCORES = 8
EPS = 1e-6
GRP = 512
OWN_GROUPS = ([0, 3, 4, 7, 8, 11, 12, 15], [1, 2, 5, 6, 9, 10, 13, 14])
N_OTHER_PROG = [1, 2, 3, 4, 5, 6, 7, 8]
N_OTHER_ACT = ([0, 2, 2, 4, 4, 6, 6, 8], [1, 1, 3, 3, 5, 5, 7, 7])
NQ = 4096 + 256
NKT = 66
SBASE = 8192
SSLOT = 1152
NK = SBASE + 4 * SSLOT
NB = NK // 128
ATTN_SCALE = 96 ** -0.5
NEG = -30000.0
MBLK = 256
NBLK = (2 * NQ) // MBLK + 32
NTHR = (2 * NQ) // MBLK
MT = MBLK // 128
C_THR = 32
C_BIO = C_THR + NTHR
C_PID = C_BIO + NBLK
CSTW = C_PID + 1
NSLOT = NBLK * MBLK
NSKIP = 14
assert 2 * MBLK <= 512


class Tr:
    __slots__ = ("w", "r", "dsem", "dcnt", "name")

    def __init__(self, name=""):
        self.w = {}
        self.r = {}
        self.dsem = None
        self.dcnt = 0
        self.name = name


class Eng:
    def __init__(self, K, name, h, self_sync=True):
        self.name = name
        self.h = h
        self.sem = K.es.enter_context(K.nc.semaphore("s_" + name))
        self.cnt = 0
        self.known = {}
        self.self_sync = self_sync


class K:
    def __init__(self, nc, es):
        self.nc = nc
        self.es = es
        self.pe = Eng(self, "pe", nc.tensor, self_sync=False)
        self.dve = Eng(self, "dve", nc.vector)
        self.act = Eng(self, "act", nc.scalar)
        self.pool = Eng(self, "pool", nc.gpsimd)
        self.sp = Eng(self, "sp", nc.sync)
        self.engs = [self.pe, self.dve, self.act, self.pool, self.sp]
        self.dsems = []
        self.nsem = 0

    def _waits(self, eng, reads, writes):
        need = {}
        for t in reads:
            for k, tok in t.w.items():
                if k not in need or need[k][1] < tok[1]:
                    need[k] = tok
        for t in writes:
            for dd in (t.w, t.r):
                for k, tok in dd.items():
                    if k not in need or need[k][1] < tok[1]:
                        need[k] = tok
        for k, (sem, val) in need.items():
            if k == eng.name and not eng.self_sync:
                continue
            if eng.known.get(k, 0) >= val:
                continue
            eng.h.wait_ge(sem, val)
            eng.known[k] = val

    def op(self, eng, fn, reads=(), writes=(), inc=True):
        ex = [t for t in reads if t.name.startswith("ps")]
        if ex:
            reads = [t for t in reads if not t.name.startswith("ps")]
            writes = list(writes) + ex
        self._waits(eng, reads, writes)
        ins = fn()
        tok = (eng.sem, eng.cnt + 1)
        if inc:
            ins.then_inc(eng.sem, 1)
            eng.cnt += 1
        for t in reads:
            t.r[eng.name] = tok
        for t in writes:
            t.w[eng.name] = tok
        return ins

    def dma(self, eng, out, in_, reads=(), writes=(), waw=True, indirect=None, **kw):
        self._waits(eng, reads, writes if waw else ())
        if indirect is not None:
            ins = eng.h.indirect_dma_start(out=out, out_offset=indirect[0], in_=in_, in_offset=indirect[1], **kw)
        else:
            ins = eng.h.dma_start(out=out, in_=in_, **kw)
        t0 = writes[0] if (writes and (waw or not reads)) else reads[0]
        if t0.dsem is None:
            t0.dsem = self.es.enter_context(self.nc.semaphore("d%d" % self.nsem))
            self.nsem += 1
            self.dsems.append(t0)
        t0.dcnt += 16
        ins.then_inc(t0.dsem, 16)
        tok = (t0.dsem, t0.dcnt)
        key = "d%d" % id(t0)
        for t in reads:
            t.r[key] = tok
        for t in writes:
            t.w[key] = tok
        return ins

    def barrier(self):
        for e in self.engs:
            for o in self.engs:
                if o is e or o.cnt == 0:
                    continue
                if e.known.get(o.name, 0) < o.cnt:
                    e.h.wait_ge(o.sem, o.cnt)
                    e.known[o.name] = o.cnt
            for t in self.dsems:
                key = "d%d" % id(t)
                if e.known.get(key, 0) < t.dcnt:
                    e.h.wait_ge(t.dsem, t.dcnt)
                    e.known[key] = t.dcnt


def dap(handle, offset, pat):
    return bass.AP(handle, offset, [list(p) for p in pat])


def build_program(phases=(1, 2, 3, 4)):
    nc = bass.Bass("TRN2", target_bir_lowering=False)

    def din(name, shape, dt=F32):
        return nc.dram_tensor(name, list(shape), dt, kind="ExternalInput")

    def dout(name, shape, dt=F32):
        return nc.dram_tensor(name, list(shape), dt, kind="ExternalOutput")

    xk = din("xk", [NKT * 128, D])
    xh = din("xh", [16, D])
    latp = din("latp", [4096, 256])
    krp = din("krp", [4096, 32])
    sconv = din("sconv", [4, 2, 512])
    pk = din("pk", [NQ, 256])
    tabk = din("tabk", [NKT * 128, 64])
    tabq = din("tabq", [64, NQ])
    maskb = din("maskb", [128, 8])
    ident_d = din("ident", [128, 128])
    bq_d = din("bq", [128, 128])
    umat_d = din("umat", [128, 128])
    cst_d = din("cst", [128, CSTW])
    g_mix = din("g_mix", [1, D]); w_in = din("w_in", [D, 4128]); g_cq = din("g_cq", [1, 256])
    w_uq = din("w_uq", [256, 768]); g_qn = din("g_qn", [1, 64]); g_qr = din("g_qr", [1, 32])
    g_ckv = din("g_ckv", [1, 256]); w_ukv = din("w_ukv", [256, 1024]); g_kn = din("g_kn", [1, 64])
    g_kr = din("g_kr", [1, 32]); w_oa = din("w_oa", [512, D]); conv_w = din("conv_w", [3, 512])
    w_oc = din("w_oc", [512, D]); w_o = din("w_o", [D, D]); g_ffn = din("g_ffn", [1, D])
    w_rg = din("w_rg", [D, 4]); b_rg = din("b_rg", [1, 4]); w_re = din("w_re", [D, 32])
    b_re = din("b_re", [1, 32]); w1 = din("w1", [32, D, 256]); w3 = din("w3", [32, D, 256])
    w2 = din("w2", [32, 256, D]); g_ple = din("g_ple", [1, D]); w_pg = din("w_pg", [D, D])
    w_ple = din("w_ple", [256, D])

    y_o = dout("y_o", [NQ, D])
    lat_o = dout("lat_o", [NQ, 256])
    kpe_o = dout("kpe_o", [NQ, 32])
    conv_o = dout("conv_o", [5, 2, 512])
    attn_dbg = dout("attn_dbg", [128, 36, 512]) if DBG.get("attn_dbg", 0) else None
    x1s = nc.dram_tensor("x1s", [NQ, D], F32, kind="Internal")
    h2tm = nc.dram_tensor("h2tm", [NQ, D], BF16, kind="Internal")
    hs = nc.dram_tensor("hs", [NSLOT, D], BF16, kind="Internal")
    ys = nc.dram_tensor("ys", [NSLOT, D], F32, kind="Internal")

    es = ExitStack()
    with es:
        Kx = K(nc, es)
        pe, dve, act, pool, sp = Kx.pe, Kx.dve, Kx.act, Kx.pool, Kx.sp
        op, dma = Kx.op, Kx.dma

        def sb(name, shape, dt, stack=es):
            return stack.enter_context(nc.sbuf_tensor("sb_" + name, list(shape), dt))

        PS = [es.enter_context(nc.psum_tensor("ps%d" % i, [128, 512], F32)) for i in range(8)]
        TPS = [Tr("ps%d" % i) for i in range(8)]

        def psb(i):
            return PS[i][:].bitcast(BF16)

        ident = sb("ident", [128, 128], BF16); t_ident = Tr()
        dma(pool, ident[:], ident_d.ap(), writes=[t_ident])
        bqm = sb("bqm", [128, 128], BF16); t_bq = Tr()
        dma(pool, bqm[:], bq_d.ap(), writes=[t_bq])
        epsb = sb("epsb", [128, 1], F32); t_eps = Tr()
        op(dve, lambda: nc.vector.memset(epsb[:], EPS), writes=[t_eps])

        def bcast_load(name, src, n, stack=es):
            t = sb(name, [128, n], F32, stack)
            tr = Tr(name)
            dma(sp, t[:], dap(src, 0, [[0, 128], [1, n]]), writes=[tr])
            return t, tr

        def colvec_load(dst_ap, src, n, off, tr):
            dma(sp, dst_ap, dap(src, off, [[1, n], [1, 1]]), writes=[tr])

        def wload(dst, src, row0, kc, ncol_total, col0, ncols, tr, dst_col0=0):
            for c in range(kc):
                dma(pool, dst[:, c, dst_col0:dst_col0 + ncols],
                    dap(src, (row0 + c * 128) * ncol_total + col0, [[ncol_total, 128], [1, ncols]]),
                    writes=[tr], waw=False)

        def rstd_from_ss(ss_ap, out_ap, n, tr_in, tr_out, tmp_ap, tr_tmp):
            op(act, lambda: nc.scalar.activation(out=tmp_ap, in_=ss_ap, func=AF.Sqrt,
                                                 bias=epsb[:ss_ap.shape[0], 0:1], scale=1.0 / n),
               reads=[tr_in, t_eps], writes=[tr_tmp])
            op(dve, lambda: nc.vector.reciprocal(out=out_ap, in_=tmp_ap), reads=[tr_tmp], writes=[tr_out])

        t_x1s = Tr(); t_h2tm = Tr(); t_hsz = Tr()
        rinfo = sb("rinfo", [128, 34, 8], F32); t_ri = [Tr() for _ in range(34)]
        base = sb("base", [128, 32], F32); t_base = Tr()
        umat = sb("umat", [128, 128], BF16); t_umat = Tr()
        dma(pool, umat[:], umat_d.ap(), writes=[t_umat])
        onesm = sb("onesm", [128, 128], BF16); t_onesm = Tr()
        op(pool, lambda: nc.gpsimd.memset(onesm[:], 1.0), writes=[t_onesm])
        cst = sb("cst", [128, CSTW], F32); t_cst = Tr()
        dma(sp, cst[:], cst_d.ap(), writes=[t_cst])
        op(dve, lambda: nc.vector.memset(base[:], 0.0), writes=[t_base])
        stA = ExitStack()
        attn = sb("attn", [128, 36, 512], BF16, stA)
        t_attn = [Tr() for _ in range(36)]
        ph12 = ExitStack()
        latT = sb("latT", [128, 2, NK], BF16, ph12); t_latT = [Tr() for _ in range(NB)]
        kext = sb("kext", [128, NK], BF16, ph12); t_krope = [Tr() for _ in range(NB)]
        cqnT = sb("cqnT", [128, 2, NQ], BF16, ph12); t_cqnT = [Tr() for _ in range(NQ // 128)]

        if 1 in phases:
            p1 = ExitStack()
            gmix_t, t_gmix = bcast_load("gmix", g_mix, D, p1)
            gcq_t, t_gcq = bcast_load("gcq", g_cq, 256, p1)
            gckv_t, t_gckv = bcast_load("gckv", g_ckv, 256, p1)
            gkr_t, t_gkr = bcast_load("gkr", g_kr, 32, p1)
            wkv = sb("wkv", [128, 8, 288], BF16, p1); t_wkv = Tr()
            wload(wkv, w_in, 0, 8, 4128, 256, 288, t_wkv)
            wcq = sb("wcq", [128, 8, 256], BF16, p1); t_wcq = Tr()
            wload(wcq, w_in, 0, 8, 4128, 0, 256, t_wcq)
            NBUF = 3
            xb = [sb("xb%d" % i, [128, D], F32, p1) for i in range(NBUF)]; t_xb = [Tr() for _ in range(NBUF)]
            NTAB = 5
            tb_ = [sb("tabk%d" % i, [128, 64], F32, p1) for i in range(NTAB)]; t_tab = [Tr() for _ in range(NTAB)]
            junk = sb("junk", [128, D], BF16, p1); t_junk = Tr()
            st = [sb("st%d" % i, [128, 16], F32, p1) for i in range(NBUF)]; t_st = [[Tr() for _ in range(16)] for _ in range(NBUF)]
            hn = [sb("hn%d" % i, [128, D], BF16, p1) for i in range(2)]; t_hn = [Tr() for _ in range(2)]
            hT = [sb("hT%d" % i, [128, 8, 128], BF16, p1) for i in range(2)]; t_hT = [Tr() for _ in range(2)]
            latf = [sb("latf%d" % i, [128, 256], F32, p1) for i in range(2)]; t_latf = [Tr() for _ in range(2)]
            krn = [sb("krn%d" % i, [128, 96], F32, p1) for i in range(2)]; t_krn = [Tr() for _ in range(2)]
            kpe = [sb("kpe%d" % i, [128, 32], F32, p1) for i in range(2)]; t_kpe = [Tr() for _ in range(2)]
            tbt = [sb("tbt%d" % i, [128, 384], BF16, p1) for i in range(2)]; t_tbt = [Tr() for _ in range(2)]
            cqb = [sb("cqb%d" % i, [128, 256], BF16, p1) for i in range(2)]; t_cqb = [Tr() for _ in range(2)]
            t_lato = Tr(); t_kpeo = Tr()

            def slots_of_tile(t):
                if t < 64:
                    return [(0, 128, t * 128)]
                s0 = 2 * (t - 64)
                return [(0, 64, SBASE + s0 * SSLOT + 1024), (64, 64, SBASE + (s0 + 1) * SSLOT + 1024)]

            def is_own(t):
                return t < 32 or t >= 64

            def own_index(t):
                return t if t < 32 else t - 32

            def S1(t):
                b = t % NBUF
                dma(sp, xb[b][:], xk.ap()[t * 128:(t + 1) * 128, :], writes=[t_xb[b]])
                dma(sp, tb_[t % NTAB][:], tabk.ap()[t * 128:(t + 1) * 128, :], writes=[t_tab[t % NTAB]])
                op(act, lambda: nc.scalar.activation(out=junk[:], in_=xb[b][:], func=AF.Square,
                                                     accum_out=st[b][:, 0:1]),
                   reads=[t_xb[b]], writes=[t_junk, t_st[b][0]])
                rstd_from_ss(st[b][:, 0:1], st[b][:, 2:3], D, t_st[b][0], t_st[b][2], st[b][:, 1:2], t_st[b][1])
                h = t % 2
                op(dve, lambda: nc.vector.scalar_tensor_tensor(out=hn[h][:], in0=xb[b][:], scalar=st[b][:, 2:3],
                                                               in1=gmix_t[:], op0=ALU.mult, op1=ALU.mult),
                   reads=[t_xb[b], t_st[b][2], t_gmix], writes=[t_hn[h]])

            def S2(t):
                h = t % 2
                pb = h
                for c in range(8):
                    op(pe, lambda c=c: nc.tensor.transpose(psb(pb)[:, c * 128:(c + 1) * 128],
                                                           hn[h][:, c * 128:(c + 1) * 128], ident[:]),
                       reads=[t_hn[h], t_ident], writes=[TPS[pb]], inc=(c == 7))
                op(act, lambda: nc.scalar.copy(out=hT[h][:].rearrange("p c n -> p (c n)"), in_=psb(pb)),
                   reads=[TPS[pb]], writes=[t_hT[h]])

            def S3(t):
                h = t % 2
                pb = 2 + h
                for c in range(8):
                    op(pe, lambda c=c: nc.tensor.matmul(PS[pb][:, 0:288], hT[h][:, c, :], wkv[:, c, :],
                                                        start=(c == 0), stop=(c == 7)),
                       reads=[t_hT[h], t_wkv], writes=[TPS[pb]], inc=(c == 7))
                if is_own(t):
                    cb = 4 if h == 0 else 7
                    for c in range(8):
                        op(pe, lambda c=c: nc.tensor.matmul(PS[cb][:, 0:256], hT[h][:, c, :], wcq[:, c, :],
                                                            start=(c == 0), stop=(c == 7)),
                           reads=[t_hT[h], t_wcq], writes=[TPS[cb]], inc=(c == 7))

            def S4a(t):
                h = t % 2
                b = t % NBUF
                pb = 2 + h
                s = st[b]; ts_ = t_st[b]
                own = is_own(t)
                cb = 4 if h == 0 else 7
                op(act, lambda: nc.scalar.activation(out=junk[:, 0:256], in_=PS[pb][:, 0:256], func=AF.Square, accum_out=s[:, 3:4]),
                   reads=[TPS[pb]], writes=[t_junk, ts_[3]])
                op(act, lambda: nc.scalar.activation(out=junk[:, 256:288], in_=PS[pb][:, 256:288], func=AF.Square, accum_out=s[:, 6:7]),
                   reads=[TPS[pb]], writes=[t_junk, ts_[6]])
                if own:
                    op(act, lambda: nc.scalar.activation(out=junk[:, 512:768], in_=PS[cb][:, 0:256], func=AF.Square, accum_out=s[:, 9:10]),
                       reads=[TPS[cb]], writes=[t_junk, ts_[9]])
                rstd_from_ss(s[:, 3:4], s[:, 5:6], 256, ts_[3], ts_[5], s[:, 4:5], ts_[4])
                rstd_from_ss(s[:, 6:7], s[:, 8:9], 32, ts_[6], ts_[8], s[:, 7:8], ts_[7])
                if own:
                    rstd_from_ss(s[:, 9:10], s[:, 11:12], 256, ts_[9], ts_[11], s[:, 10:11], ts_[10])

            def S4b(t):
                h = t % 2
                b = t % NBUF
                pb = 2 + h
                s = st[b]; ts_ = t_st[b]
                kr = krn[h]
                tbl = tb_[t % NTAB]; t_tbl = t_tab[t % NTAB]
                op(dve, lambda: nc.vector.scalar_tensor_tensor(out=latf[h][:], in0=PS[pb][:, 0:256], scalar=s[:, 5:6],
                                                               in1=gckv_t[:], op0=ALU.mult, op1=ALU.mult),
                   reads=[TPS[pb], ts_[5], t_gckv], writes=[t_latf[h]])
                op(dve, lambda: nc.vector.scalar_tensor_tensor(out=kr[:, 0:32], in0=PS[pb][:, 256:288], scalar=s[:, 8:9],
                                                               in1=gkr_t[:], op0=ALU.mult, op1=ALU.mult),
                   reads=[TPS[pb], ts_[8], t_gkr], writes=[t_krn[h]])
                if is_own(t):
                    cb = 4 if h == 0 else 7
                    op(dve, lambda: nc.vector.scalar_tensor_tensor(out=cqb[h][:], in0=PS[cb][:, 0:256], scalar=s[:, 11:12],
                                                                   in1=gcq_t[:], op0=ALU.mult, op1=ALU.mult),
                       reads=[TPS[cb], ts_[11], t_gcq], writes=[t_cqb[h]])
                op(dve, lambda: nc.vector.tensor_tensor(out=kr[:, 32:64], in0=kr[:, 0:32], in1=tbl[:, 0:32], op=ALU.mult),
                   reads=[t_krn[h], t_tbl], writes=[t_krn[h]])
                op(dve, lambda: nc.vector.tensor_tensor(out=kr[:, 64:80], in0=kr[:, 16:32], in1=tbl[:, 32:48], op=ALU.mult),
                   reads=[t_krn[h], t_tbl], writes=[t_krn[h]])
                op(dve, lambda: nc.vector.tensor_tensor(out=kr[:, 80:96], in0=kr[:, 0:16], in1=tbl[:, 48:64], op=ALU.mult),
                   reads=[t_krn[h], t_tbl], writes=[t_krn[h]])
                op(dve, lambda: nc.vector.tensor_tensor(out=kpe[h][:], in0=kr[:, 32:64], in1=kr[:, 64:96], op=ALU.add),
                   reads=[t_krn[h]], writes=[t_kpe[h]])

            def S4c(t):
                h = t % 2
                op(pool, lambda: nc.gpsimd.tensor_copy(out=tbt[h][:, 0:256], in_=latf[h][:]),
                   reads=[t_latf[h]], writes=[t_tbt[h]])
                op(pool, lambda: nc.gpsimd.tensor_copy(out=tbt[h][:, 256:384].rearrange("p (a b) -> p a b", a=4),
                                                       in_=kpe[h][:].unsqueeze(1).to_broadcast([128, 4, 32])),
                   reads=[t_kpe[h]], writes=[t_tbt[h]])
                if is_own(t):
                    oi = own_index(t)
                    dma(sp, lat_o.ap()[oi * 128:(oi + 1) * 128, :], latf[h][:], reads=[t_latf[h]], writes=[t_lato], waw=False)
                    dma(sp, kpe_o.ap()[oi * 128:(oi + 1) * 128, :], kpe[h][:], reads=[t_kpe[h]], writes=[t_kpeo], waw=False)

            def tb_transposes(h, pbank, slots, nrows=128):
                for c in range(3):
                    op(pe, lambda c=c: nc.tensor.transpose(psb(pbank)[:, c * 128:c * 128 + nrows],
                                                           tbt[h][:nrows, c * 128:(c + 1) * 128], ident[:nrows, :nrows]),
                       reads=[t_tbt[h], t_ident], writes=[TPS[pbank]], inc=(c == 2))
                for (c0, n, s0) in slots:
                    blk = s0 // 128
                    op(dve, lambda c0=c0, n=n, s0=s0: nc.vector.tensor_copy(
                        out=latT[:, :, s0:s0 + n],
                        in_=psb(pbank)[:, 0:256].rearrange("p (c n) -> p c n", c=2)[:, :, c0:c0 + n]),
                       reads=[TPS[pbank]], writes=[t_latT[blk]])
                    op(act, lambda c0=c0, n=n, s0=s0: nc.scalar.copy(
                        out=kext[64:128, s0:s0 + n], in_=psb(pbank)[64:128, 256 + c0:256 + c0 + n]),
                       reads=[TPS[pbank]], writes=[t_krope[blk]])

            def S5(t):
                h = t % 2
                tb_transposes(h, 5, slots_of_tile(t))
                if is_own(t):
                    oi = own_index(t)
                    for c in range(2):
                        op(pe, lambda c=c: nc.tensor.transpose(psb(6)[:, c * 128:(c + 1) * 128],
                                                               cqb[h][:, c * 128:(c + 1) * 128], ident[:]),
                           reads=[t_cqb[h], t_ident], writes=[TPS[6]], inc=(c == 1))
                    op(dve, lambda: nc.vector.tensor_copy(
                        out=cqnT[:, :, oi * 128:(oi + 1) * 128],
                        in_=psb(6)[:, 0:256].rearrange("p (c n) -> p c n", c=2)),
                       reads=[TPS[6]], writes=[t_cqnT[oi]])

            for s_ in range(4):
                s0 = SBASE + s_ * SSLOT + 1088
                op(pool, lambda s0=s0: nc.gpsimd.memset(latT[:, :, s0:s0 + 64], 0.0), writes=[t_latT[s0 // 128]])
                op(pool, lambda s0=s0: nc.gpsimd.memset(kext[64:128, s0:s0 + 64], 0.0), writes=[t_krope[s0 // 128]])

            stages = [S1, S2, S3, S4a, S4b, S4c, S5]
            nkt = DBG.get("nkt", NKT)
            for step in range(nkt + len(stages) - 1):
                for si in reversed(range(len(stages))):
                    t = step - si
                    if 0 <= t < nkt:
                        stages[si](t)

            krpb = [sb("krpb%d" % i, [128, 32], BF16, p1) for i in range(2)]; t_krpb = [Tr() for _ in range(2)]
            for j in range(DBG.get("npast", 32)):
                h = j % 2
                s_, r = divmod(j, 8)
                dma(pool, tbt[h][:, 0:256], latp.ap()[j * 128:(j + 1) * 128, :], writes=[t_tbt[h]])
                dma(pool, krpb[h][:], krp.ap()[j * 128:(j + 1) * 128, :], writes=[t_krpb[h]])
                op(dve, lambda: nc.vector.tensor_copy(out=tbt[h][:, 256:384].rearrange("p (a b) -> p a b", a=4),
                                                      in_=krpb[h][:].unsqueeze(1).to_broadcast([128, 4, 32])),
                   reads=[t_krpb[h]], writes=[t_tbt[h]])
                tb_transposes(h, 5 + (j % 2) * 2, [(0, 128, SBASE + s_ * SSLOT + r * 128)])
            Kx.barrier()
            p1.close()

        if 2 in phases:
            p2 = ExitStack()
            wkn = sb("wkn", [128, 2, 512], BF16, p2); t_wkn = Tr()
            wv = sb("wv", [128, 2, 512], BF16, p2); t_wv = Tr()
            wq = sb("wq", [128, 2, 1024], BF16, p2); t_wq = Tr()
            for c in range(2):
                dma(pool, wkn[:, c, :].rearrange("p (h d) -> p h d", h=8),
                    dap(w_ukv, c * 128 * 1024, [[1024, 128], [128, 8], [1, 64]]), writes=[t_wkn])
                dma(pool, wv[:, c, :].rearrange("p (h d) -> p h d", h=8),
                    dap(w_ukv, c * 128 * 1024 + 64, [[1024, 128], [128, 8], [1, 64]]), writes=[t_wv])
                wq3 = wq[:, c, :].rearrange("p (h d) -> p h d", h=8)
                for (so, n, do) in ((0, 64, 0), (64, 32, 64), (80, 16, 96), (64, 16, 112)):
                    dma(pool, wq3[:, :, do:do + n],
                        dap(w_uq, c * 128 * 768 + so, [[768, 128], [96, 8], [1, n]]), writes=[t_wq])
            gq = sb("gq", [128, 1], F32, p2); t_gq = Tr()
            colvec_load(gq[0:64, :], g_qn, 64, 0, t_gq)
            colvec_load(gq[64:96, :], g_qr, 32, 0, t_gq)
            colvec_load(gq[96:112, :], g_qr, 16, 16, t_gq)
            colvec_load(gq[112:128, :], g_qr, 16, 0, t_gq)
            gkn = sb("gkn", [64, 1], F32, p2); t_gkn = Tr()
            colvec_load(gkn[:, :], g_kn, 64, 0, t_gkn)
            mkb = sb("mkb", [128, 8], F32, p2); t_mkb = Tr()
            dma(sp, mkb[:], maskb.ap(), writes=[t_mkb])
            tq = sb("tq", [128, NQ], F32, p2); t_tq = Tr()
            dma(sp, tq[64:128, :], tabq.ap(), writes=[t_tq])
            V = sb("V", [128, NB, 65], BF16, p2); t_V = [Tr() for _ in range(13)]
            for g in range(13):
                b1 = min(NB, g * 8 + 8)
                op(pool, lambda g=g, b1=b1: nc.gpsimd.memset(V[:, g * 8:b1, 64:65], 1.0), writes=[t_V[g]])
            qext = sb("qext", [128, NQ], BF16, p2); t_q = [Tr() for _ in range(9)]
            t_kn = [Tr() for _ in range(25)]
            sqb = [sb("sqb%d" % i, [128, 512], BF16, p2) for i in range(2)]; t_sqb = [Tr() for _ in range(2)]
            rsf = [sb("rsf%d" % i, [128, 512], F32, p2) for i in range(2)]; t_rsf = [Tr() for _ in range(2)]
            qtm = [sb("qtm%d" % i, [128, 512], F32, p2) for i in range(2)]; t_qtm = [Tr() for _ in range(2)]
            NSB = 4
            pT = [sb("pT%d" % i, [128, 512], BF16, p2) for i in range(NSB)]; t_pT = [Tr() for _ in range(NSB)]
            rcp = sb("rcp", [128, 8], F32, p2); t_rcp = [Tr() for _ in range(8)]

            qgroups = [(i * 512, 512) for i in range(8)] + [(4096, 256)]
            cnt_prep = [0]

            def prep_head(h):
                jobs = [("k", g, 64, 512) for g in range(25)] + [("q", gi, 128, n) for gi, (c0, n) in enumerate(qgroups)]
                nj = len(jobs)

                def J1(i):
                    kind, g, P, n = jobs[i]
                    pm = i % 5
                    for c in range(2):
                        if kind == "k":
                            op(pe, lambda c=c: nc.tensor.matmul(PS[pm][0:64, :], wkn[:, c, h * 64:(h + 1) * 64],
                                                                latT[:, c, g * 512:(g + 1) * 512], start=(c == 0), stop=(c == 1)),
                               reads=[t_wkn] + t_latT[g * 4:g * 4 + 4], writes=[TPS[pm]], inc=(c == 1))
                        else:
                            c0 = qgroups[g][0]
                            op(pe, lambda c=c: nc.tensor.matmul(PS[pm][:, :n], wq[:, c, h * 128:(h + 1) * 128],
                                                                cqnT[:, c, c0:c0 + n], start=(c == 0), stop=(c == 1)),
                               reads=[t_wq] + t_cqnT[c0 // 128:(c0 + n) // 128], writes=[TPS[pm]], inc=(c == 1))

                def J2(i):
                    kind, g, P, n = jobs[i]
                    pm = i % 5; k = i % 2
                    op(act, lambda: nc.scalar.activation(out=sqb[k][:P, :n], in_=PS[pm][:P, :n], func=AF.Square),
                       reads=[TPS[pm]], writes=[t_sqb[k]])

                def J3(i):
                    kind, g, P, n = jobs[i]
                    pst = 5 + i % 3; k = i % 2
                    bmat = bqm[0:64, 0:64] if kind == "k" else bqm[:, :]
                    op(pe, lambda: nc.tensor.matmul(PS[pst][:P, :n], bmat, sqb[k][:P, :n], start=True, stop=True),
                       reads=[t_sqb[k], t_bq], writes=[TPS[pst]])

                def J4(i):
                    kind, g, P, n = jobs[i]
                    pst = 5 + i % 3; k = i % 2
                    op(act, lambda: nc.scalar.activation(out=rsf[k][:P, :n], in_=PS[pst][:P, :n], func=AF.Ln,
                                                         bias=epsb[:P, 0:1], scale=1.0),
                       reads=[TPS[pst], t_eps], writes=[t_rsf[k]])
                    op(act, lambda: nc.scalar.activation(out=rsf[k][:P, :n], in_=rsf[k][:P, :n], func=AF.Exp, scale=-0.5),
                       reads=[t_rsf[k]], writes=[t_rsf[k]])

                def J5(i):
                    kind, g, P, n = jobs[i]
                    pm = i % 5; k = i % 2
                    if kind == "k":
                        op(dve, lambda: nc.vector.scalar_tensor_tensor(out=kext[0:64, g * 512:(g + 1) * 512], in0=PS[pm][0:64, :],
                                                                       scalar=gkn[:, 0:1], in1=rsf[k][0:64, :],
                                                                       op0=ALU.mult, op1=ALU.mult),
                           reads=[TPS[pm], t_gkn, t_rsf[k]], writes=[t_kn[g]])
                    else:
                        c0 = qgroups[g][0]
                        op(dve, lambda: nc.vector.scalar_tensor_tensor(out=qext[0:64, c0:c0 + n], in0=PS[pm][0:64, :n],
                                                                       scalar=gq[0:64, 0:1], in1=rsf[k][0:64, :n],
                                                                       op0=ALU.mult, op1=ALU.mult),
                           reads=[TPS[pm], t_gq, t_rsf[k]], writes=[t_q[g]])
                        op(dve, lambda: nc.vector.scalar_tensor_tensor(out=qtm[k][64:128, :n], in0=PS[pm][64:128, :n],
                                                                       scalar=gq[64:128, 0:1], in1=rsf[k][64:128, :n],
                                                                       op0=ALU.mult, op1=ALU.mult),
                           reads=[TPS[pm], t_gq, t_rsf[k]], writes=[t_qtm[k]])
                        op(pool, lambda: nc.gpsimd.tensor_tensor(out=qext[64:128, c0:c0 + n], in0=qtm[k][64:128, :n],
                                                                 in1=tq[64:128, c0:c0 + n], op=ALU.mult),
                           reads=[t_qtm[k], t_tq], writes=[t_q[g]])

                jst = [J1, J2, J3, J4, J5]
                for step in range(nj + len(jst) - 1):
                    for si, fn in enumerate(jst):
                        i = step - si
                        if 0 <= i < nj:
                            fn(i)
                for g in range(13):
                    b0 = g * 8
                    b1 = min(NB, b0 + 8)
                    pb = g % 2
                    for bi in range(b0, b1):
                        for c in range(2):
                            op(pe, lambda bi=bi, c=c: nc.tensor.matmul(PS[pb][:, (bi - b0) * 64:(bi - b0 + 1) * 64],
                                                                       latT[:, c, bi * 128:(bi + 1) * 128],
                                                                       wv[:, c, h * 64:(h + 1) * 64], start=(c == 0), stop=(c == 1)),
                               reads=[t_wv, t_latT[bi]], writes=[TPS[pb]], inc=(c == 1 and bi == b1 - 1))
                    nb_ = b1 - b0
                    eng_, fn_ = (act, nc.scalar.copy) if g % 2 == 0 else (dve, nc.vector.tensor_copy)
                    op(eng_, lambda fn_=fn_: fn_(out=V[:, b0:b1, 0:64],
                                                 in_=PS[pb][:, 0:nb_ * 64].rearrange("p (b d) -> p b d", d=64)),
                       reads=[TPS[pb]], writes=[t_V[g]])

            def attn_tiles():
                items = []
                for i in range(8):
                    tiles = []
                    for j in range(i):
                        for r in range(4):
                            tiles.append((4 * j + r, 128, 0, 512, None, None))
                    nprog = N_OTHER_PROG[i]
                    for j in range(nprog):
                        for r in range(4):
                            tiles.append((32 + 4 * j + r, 128, 0, 512, (i if j == nprog - 1 else None), None))
                    for r in range(4):
                        tiles.append((4 * i + r, 128, r * 128, 512 - r * 128, None, r))
                    items.append((i, tiles, [(jq, 128) for jq in range(4)], [4 * i + jq for jq in range(4)]))
                for s_ in range(4):
                    kb0 = 64 + 9 * s_
                    tiles = [(kb0 + r, 128, 64 * s_, 64, None, None) for r in range(8)]
                    tiles.append((kb0 + 8, 64, 64 * s_, 64, None, None))
                    items.append((8, tiles, [(0, 64)], [32 + s_]))
                return items

            def attention_head(h):
                for (gi, tiles, qblocks, atiles) in attn_tiles():
                    qc0 = qgroups[gi][0]
                    nt = len(tiles)
                    single = (gi == 8)
                    for idx in range(nt + NSB - 1):
                        if idx < nt:
                            kb, nk, col0, ncols, bidx, diag = tiles[idx]
                            bank = idx % NSB
                            op(pe, lambda kb=kb, nk=nk, col0=col0, ncols=ncols, bank=bank: nc.tensor.matmul(
                                PS[bank][:nk, :ncols], kext[:, kb * 128:kb * 128 + nk],
                                qext[:, qc0 + col0:qc0 + col0 + ncols], start=True, stop=True),
                               reads=[t_kn[kb // 4], t_krope[kb], t_q[gi]], writes=[TPS[bank]])
                        t = idx - (NSB - 1)
                        if t < 0:
                            continue
                        kb, nk, col0, ncols, bidx, diag = tiles[t]
                        bank = t % NSB
                        bias = mkb[:nk, bidx:bidx + 1] if bidx is not None else 0.0
                        op(act, lambda nk=nk, ncols=ncols, bank=bank, bias=bias: nc.scalar.activation(
                            out=pT[bank][:nk, :ncols], in_=PS[bank][:nk, :ncols], func=AF.Exp,
                            bias=bias, scale=ATTN_SCALE),
                           reads=[TPS[bank], t_mkb], writes=[t_pT[bank]])
                        if diag is not None:
                            op(dve, lambda bank=bank: nc.vector.memset(pT[bank][64:128, 0:64], 0.0), writes=[t_pT[bank]])
                        if single:
                            qlist = [(0, 0, 64)]
                        else:
                            q_first = col0 // 128
                            qlist = [(jq, (jq - q_first) * 128, 128) for jq in range(q_first, 4)]
                        for li, (jq, off, m) in enumerate(qlist):
                            first = (t == 0)
                            last = (t == nt - 1) if single else (diag is not None and diag == jq)
                            op(pe, lambda jq=jq, off=off, m=m, nk=nk, kb=kb, bank=bank, first=first, last=last: nc.tensor.matmul(
                                PS[4 + jq][:m, 0:65], pT[bank][:nk, off:off + m], V[:nk, kb, :], start=first, stop=last),
                               reads=[t_pT[bank], t_V[kb // 8]], writes=[TPS[4 + jq]], inc=(li == len(qlist) - 1))
                    for (jq, m), at in zip(qblocks, atiles):
                        op(dve, lambda jq=jq, m=m: nc.vector.reciprocal(out=rcp[:m, jq:jq + 1], in_=PS[4 + jq][:m, 64:65]),
                           reads=[TPS[4 + jq]], writes=[t_rcp[jq]])
                        op(dve, lambda jq=jq, m=m, at=at: nc.vector.tensor_scalar(
                            out=attn[:m, at, h * 64:(h + 1) * 64], in0=PS[4 + jq][:m, 0:64],
                            scalar1=rcp[:m, jq:jq + 1], scalar2=None, op0=ALU.mult),
                           reads=[TPS[4 + jq], t_rcp[jq]], writes=[t_attn[at]])

            for h in range(DBG.get("nheads", 8)):
                if not DBG.get("noprep", 0):
                    prep_head(h)
                if not DBG.get("noattn", 0):
                    attention_head(h)
            Kx.barrier()
            if DBG.get("attn_dbg", 0):
                t_ad = Tr()
                dma(pool, attn_dbg.ap(), attn[:], reads=t_attn, writes=[t_ad])
            p2.close()

        Kx.barrier()
        ph12.close()

        def rms_tile(xt, t_x, hnb, t_hnb, stt, t_stt):
            op(act, lambda: nc.scalar.activation(out=hnb[:], in_=xt[:], func=AF.Square, accum_out=stt[:, 0:1]),
               reads=[t_x], writes=[t_hnb, t_stt[0]])
            rstd_from_ss(stt[:, 0:1], stt[:, 2:3], D, t_stt[0], t_stt[2], stt[:, 1:2], t_stt[1])
            op(dve, lambda: nc.vector.tensor_scalar(out=hnb[:], in0=xt[:], scalar1=stt[:, 2:3], scalar2=None, op0=ALU.mult),
               reads=[t_x, t_stt[2]], writes=[t_hnb])

        def transpose_gain(src, t_src, nrows, nch, pbank, dst_ap, t_dst, gcols, t_g, interleave=False):
            for c in range(nch):
                cs = slice(c, nch * 128, nch) if interleave else slice(c * 128, (c + 1) * 128)
                op(pe, lambda c=c, cs=cs: nc.tensor.transpose(psb(pbank)[:, c * 128:c * 128 + nrows],
                                                              src[:nrows, cs], ident[:nrows, :nrows]),
                   reads=[t_src, t_ident], writes=[TPS[pbank]], inc=(c == nch - 1))
            pv = psb(pbank)[:, 0:nch * 128].rearrange("p (c n) -> p c n", c=nch)[:, :, 0:nrows]
            if gcols is None:
                op(act, lambda: nc.scalar.copy(out=dst_ap, in_=pv), reads=[TPS[pbank]], writes=[t_dst])
            else:
                op(dve, lambda: nc.vector.tensor_tensor(out=dst_ap, in0=pv,
                                                        in1=gcols[:, 0:nch].unsqueeze(2).to_broadcast([128, nch, nrows]),
                                                        op=ALU.mult),
                   reads=[TPS[pbank], t_g], writes=[t_dst])

        def gaincols_load(name, src, stack):
            t = sb(name, [128, 8], F32, stack)
            tr = Tr(name)
            dma(sp, t[:], dap(src, 0, [[1, 128], [128, 8]]), writes=[tr], allow_slow_non_contiguous=True)
            return t, tr

        if 3 in phases:
            p3 = ExitStack()
            w3s = sb("w3in", [128, 8, 3584], BF16, p3)
            t_w3b = {"cx": Tr(), "cb": Tr(), "g": Tr()}

            def t_w3_of(woff):
                return t_w3b["cb"] if woff < 512 else (t_w3b["cx"] if woff < 1536 else t_w3b["g"])

            wload(w3s, w_in, 0, 8, 4128, 544 + 512, 1024, t_w3b["cx"], dst_col0=512)
            wload(w3s, w_in, 0, 8, 4128, 544, 512, t_w3b["cb"], dst_col0=0)
            wload(w3s, w_in, 0, 8, 4128, 544 + 1536, 2048, t_w3b["g"], dst_col0=1536)
            woc = sb("woc", [128, 4, D], BF16, p3); t_woc = Tr()
            wload(woc, w_oc, 0, 4, D, 0, D, t_woc)
            woa = sb("woa", [128, 4, D], BF16, p3); t_woa = Tr()
            wload(woa, w_oa, 0, 4, D, 0, D, t_woa)
            wo = sb("wo", [128, 8, D], BF16, p3); t_wo = Tr()
            wload(wo, w_o, 0, 8, D, 0, D, t_wo)
            wr = sb("wr", [128, 8, 36], BF16, p3); t_wr = Tr()
            wload(wr, w_rg, 0, 8, 4, 0, 4, t_wr)
            wload(wr, w_re, 0, 8, 32, 0, 32, t_wr, dst_col0=4)
            gmixc, t_gmixc = gaincols_load("gmixc", g_mix, p3)
            gffn_t, t_gffn = bcast_load("gffn_t", g_ffn, D, p3)
            selb = [sb("selb%d" % i, [128, 32], BF16, p3) for i in range(2)]; t_selb = [Tr() for _ in range(2)]
            zt = sb("zt", [128, 2048], BF16, p3); t_zt = Tr()
            op(pool, lambda: nc.gpsimd.memset(zt[:], 0.0), writes=[t_zt])
            hs_v = hs.ap().rearrange("(p r) d -> p (r d)", p=128)
            ZC = 2048
            zfill = [(z0, min(ZC, (NSLOT // 128) * D - z0)) for z0 in range(0, (NSLOT // 128) * D, ZC)]

            def zero_fill_some(k_):
                for _ in range(k_):
                    if zfill:
                        z0, zn = zfill.pop(0)
                        dma(sp, hs_v[:, z0:z0 + zn], zt[:, 0:zn], reads=[t_zt], writes=[t_hsz], waw=False)
            brt = sb("brt", [128, 36], F32, p3); t_brt = Tr()
            dma(sp, brt[:, 0:4], dap(b_rg, 0, [[0, 128], [1, 4]]), writes=[t_brt])
            dma(sp, brt[:, 4:36], dap(b_re, 0, [[0, 128], [1, 32]]), writes=[t_brt])
            cw = sb("cw", [128, 4, 3], F32, p3); t_cw = Tr()
            for k_ in range(3):
                dma(sp, cw[:, :, k_], dap(conv_w, k_ * 512, [[1, 128], [128, 4]]), writes=[t_cw], allow_slow_non_contiguous=True)
            xt3 = [sb("x3_%d" % i, [128, D], F32, p3) for i in range(4)]; t_x3 = [Tr() for _ in range(4)]
            hn3 = [sb("hn3_%d" % i, [128, D], BF16, p3) for i in range(2)]; t_hn3 = [Tr() for _ in range(2)]
            st3 = [sb("st3_%d" % i, [128, 4], F32, p3) for i in range(4)]; t_st3 = [[Tr() for _ in range(4)] for _ in range(4)]
            hT3 = sb("hT3", [128, 8, 512], BF16, p3); t_hT3 = [Tr() for _ in range(4)]
            upad = sb("upad", [128, 4, 516], F32, p3); t_up = [Tr() for _ in range(4)]
            uhalo = sb("uhalo", [128, 4, 16], F32, p3); t_uh = Tr()
            cc = sb("cc", [128, 512], F32, p3); t_cc = Tr()
            cv = sb("cv", [128, 512], F32, p3); t_cv = Tr()
            bc = sb("bc", [128, 4, 512], BF16, p3); t_bc = [Tr() for _ in range(4)]
            tm = [sb("tm%d" % i, [128, 512], F32, p3) for i in range(2)]; t_tm = [Tr() for _ in range(2)]
            mT = sb("mT", [128, 8, 512], BF16, p3); t_mT = [Tr() for _ in range(8)]
            aT = sb("aT", [128, 4, 512], BF16, p3); t_aT = [Tr() for _ in range(4)]
            rt = [sb("rt%d" % i, [128, 160], F32, p3) for i in range(2)]; t_rt = [Tr() for _ in range(2)]
            t_convo = Tr()
            bank_rr = [0]

            def nbank():
                b = 2 + (bank_rr[0] % 4)
                bank_rr[0] += 1
                return b

            def fm_matmul(bank, wt, t_w, woff, rhs_of_kc, nk, n, reads):
                for kc in range(nk):
                    op(pe, lambda kc=kc: nc.tensor.matmul(PS[bank][:, :n], wt[:, kc, woff:woff + 128], rhs_of_kc(kc),
                                                          start=(kc == 0), stop=(kc == nk - 1)),
                       reads=[t_w] + reads, writes=[TPS[bank]], inc=(kc == nk - 1))

            dma(sp, xt3[0][0:16, :], xh.ap(), writes=[t_x3[0]])
            op(act, lambda: nc.scalar.activation(out=hn3[0][0:16, :], in_=xt3[0][0:16, :], func=AF.Square,
                                                 accum_out=st3[0][0:16, 0:1]),
               reads=[t_x3[0]], writes=[t_hn3[0], t_st3[0][0]])
            rstd_from_ss(st3[0][0:16, 0:1], st3[0][0:16, 2:3], D, t_st3[0][0], t_st3[0][2], st3[0][0:16, 1:2], t_st3[0][1])
            op(dve, lambda: nc.vector.tensor_scalar(out=hn3[0][0:16, :], in0=xt3[0][0:16, :], scalar1=st3[0][0:16, 2:3],
                                                    scalar2=None, op0=ALU.mult),
               reads=[t_x3[0], t_st3[0][2]], writes=[t_hn3[0]])
            transpose_gain(hn3[0], t_hn3[0], 16, 8, 0, hT3[:, :, 0:16], t_hT3[0], gmixc, t_gmixc)
            for c in range(4):
                b1 = nbank()
                fm_matmul(b1, w3s, t_w3_of(512), 512 + c * 128, lambda kc: hT3[:, kc, 0:16], 8, 16, [t_hT3[0]])
                op(act, lambda b1=b1: nc.scalar.copy(out=cc[:, 0:16], in_=PS[b1][:, 0:16]), reads=[TPS[b1]], writes=[t_cc])
                b2 = nbank()
                fm_matmul(b2, w3s, t_w3_of(1024), 1024 + c * 128, lambda kc: hT3[:, kc, 0:16], 8, 16, [t_hT3[0]])
                op(dve, lambda c=c, b2=b2: nc.vector.tensor_tensor(out=uhalo[:, c, :], in0=PS[b2][:, 0:16], in1=cc[:, 0:16], op=ALU.mult),
                   reads=[TPS[b2], t_cc], writes=[t_uh])

            n_groups3 = DBG.get("ngroups3", 9)
            for g in range(n_groups3):
                samp = (g == 8)
                ntile = 2 if samp else 4
                n = ntile * 128
                q0 = g * 512
                nseq, L = (4, 64) if samp else (1, 512)
                up4 = upad[:, :, 0:nseq * (L + 2)].rearrange("p c (s l) -> p c s l", s=nseq)
                for j in range(ntile):
                    dma(sp, xt3[j][:], xk.ap()[(64 * 128 + j * 128 if samp else q0 + j * 128):(64 * 128 + j * 128 if samp else q0 + j * 128) + 128, :],
                        writes=[t_x3[j]])
                    rms_tile(xt3[j], t_x3[j], hn3[j % 2], t_hn3[j % 2], st3[j], t_st3[j])
                    transpose_gain(hn3[j % 2], t_hn3[j % 2], 128, 8, j % 2, hT3[:, :, j * 128:(j + 1) * 128], t_hT3[j], gmixc, t_gmixc)
                hreads = t_hT3[0:ntile]
                if samp:
                    for s_ in range(4):
                        transpose_gain(attn[:, 32 + s_, :], t_attn[32 + s_], 64, 4, s_ % 2, aT[:, :, s_ * 64:(s_ + 1) * 64], t_aT[s_], None, None)
                else:
                    for j in range(4):
                        transpose_gain(attn[:, g * 4 + j, :], t_attn[g * 4 + j], 128, 4, j % 2, aT[:, :, j * 128:(j + 1) * 128], t_aT[j], None, None)
                for c in range(4):
                    if samp:
                        for k_ in range(2):
                            dma(sp, up4[:, c, :, k_], dap(sconv, c * 128 + k_ * 512, [[1, 128], [1024, 4]]),
                                writes=[t_up[c]], allow_slow_non_contiguous=True)
                    else:
                        op(pool, lambda c=c: nc.gpsimd.tensor_copy(out=upad[:, c, 0:2], in_=uhalo[:, c, 2 * g:2 * g + 2]),
                           reads=[t_uh], writes=[t_up[c]])
                    b1 = nbank()
                    fm_matmul(b1, w3s, t_w3_of(512), 512 + c * 128, lambda kc: hT3[:, kc, 0:n], 8, n, hreads)
                    op(act, lambda b1=b1: nc.scalar.copy(out=cc[:, 0:n], in_=PS[b1][:, 0:n]), reads=[TPS[b1]], writes=[t_cc])
                    b2 = nbank()
                    fm_matmul(b2, w3s, t_w3_of(1024), 1024 + c * 128, lambda kc: hT3[:, kc, 0:n], 8, n, hreads)
                    op(dve, lambda c=c, b2=b2: nc.vector.tensor_tensor(
                        out=up4[:, c, :, 2:2 + L], in0=PS[b2][:, 0:n].rearrange("p (s l) -> p s l", s=nseq),
                        in1=cc[:, 0:n].rearrange("p (s l) -> p s l", s=nseq), op=ALU.mult),
                       reads=[TPS[b2], t_cc], writes=[t_up[c]])
                    cv3 = cv[:, 0:n].rearrange("p (s l) -> p s l", s=nseq)
                    op(dve, lambda c=c: nc.vector.tensor_scalar(out=cv3, in0=up4[:, c, :, 0:L], scalar1=cw[:, c, 0:1],
                                                                scalar2=None, op0=ALU.mult),
                       reads=[t_up[c], t_cw], writes=[t_cv])
                    for k_ in (1, 2):
                        op(dve, lambda c=c, k_=k_: nc.vector.scalar_tensor_tensor(
                            out=cv3, in0=up4[:, c, :, k_:k_ + L], scalar=cw[:, c, k_:k_ + 1], in1=cv3,
                            op0=ALU.mult, op1=ALU.add),
                           reads=[t_up[c], t_cw, t_cv], writes=[t_cv])
                    b3 = nbank()
                    fm_matmul(b3, w3s, t_w3_of(0), 0 + c * 128, lambda kc: hT3[:, kc, 0:n], 8, n, hreads)
                    op(dve, lambda c=c, b3=b3: nc.vector.tensor_tensor(out=bc[:, c, 0:n], in0=PS[b3][:, 0:n], in1=cv[:, 0:n], op=ALU.mult),
                       reads=[TPS[b3], t_cv], writes=[t_bc[c]])
                    if samp:
                        for k_ in range(2):
                            dma(sp, dap(conv_o, 1024 + c * 128 + k_ * 512, [[1, 128], [1024, 4]]), up4[:, c, :, L + k_],
                                reads=[t_up[c]], writes=[t_convo], allow_slow_non_contiguous=True)
                    elif g == 7:
                        dma(sp, dap(conv_o, c * 128, [[1, 128], [512, 2]]), upad[:, c, L:L + 2],
                            reads=[t_up[c]], writes=[t_convo], allow_slow_non_contiguous=True)
                for c in range(8):
                    bg = nbank()
                    fm_matmul(bg, w3s, t_w3_of(2560), 2560 + c * 128, lambda kc: hT3[:, kc, 0:n], 8, n, hreads)
                    op(act, lambda bg=bg: nc.scalar.activation(out=tm[0][:, 0:n], in_=PS[bg][:, 0:n], func=AF.Sigmoid),
                       reads=[TPS[bg]], writes=[t_tm[0]])
                    by = nbank()
                    fm_matmul(by, woc, t_woc, c * 128, lambda kc: bc[:, kc, 0:n], 4, n, t_bc)
                    op(dve, lambda by=by: nc.vector.tensor_tensor(out=tm[0][:, 0:n], in0=PS[by][:, 0:n], in1=tm[0][:, 0:n], op=ALU.mult),
                       reads=[TPS[by], t_tm[0]], writes=[t_tm[0]])
                    bg2 = nbank()
                    fm_matmul(bg2, w3s, t_w3_of(1536), 1536 + c * 128, lambda kc: hT3[:, kc, 0:n], 8, n, hreads)
                    op(act, lambda bg2=bg2: nc.scalar.activation(out=tm[1][:, 0:n], in_=PS[bg2][:, 0:n], func=AF.Sigmoid),
                       reads=[TPS[bg2]], writes=[t_tm[1]])
                    by2 = nbank()
                    fm_matmul(by2, woa, t_woa, c * 128, lambda kc: aT[:, kc, 0:n], 4, n, t_aT)
                    op(dve, lambda by2=by2: nc.vector.tensor_tensor(out=tm[1][:, 0:n], in0=PS[by2][:, 0:n], in1=tm[1][:, 0:n], op=ALU.mult),
                       reads=[TPS[by2], t_tm[1]], writes=[t_tm[1]])
                    op(pool, lambda c=c: nc.gpsimd.tensor_tensor(out=mT[:, c, 0:n], in0=tm[0][:, 0:n], in1=tm[1][:, 0:n], op=ALU.add),
                       reads=[t_tm[0], t_tm[1]], writes=[t_mT[c]])
                zero_fill_some((len(zfill) + (n_groups3 - g) - 1) // (n_groups3 - g))
                brd = {}
                def E1(j):
                    ti = g * 4 + j
                    ti = g * 4 + j
                    for hf in range(2):
                        for kc in range(8):
                            op(pe, lambda kc=kc, hf=hf, j=j: nc.tensor.matmul(PS[6 + hf][:, :], mT[:, kc, j * 128:(j + 1) * 128],
                                                                            wo[:, kc, hf * 512:(hf + 1) * 512],
                                                                            start=(kc == 0), stop=(kc == 7)),
                               reads=[t_wo] + t_mT, writes=[TPS[6 + hf]], inc=(kc == 7))
                        op(dve, lambda hf=hf, j=j: nc.vector.tensor_tensor(out=xt3[j][:, hf * 512:(hf + 1) * 512], in0=PS[6 + hf][:, :],
                                                                          in1=xt3[j][:, hf * 512:(hf + 1) * 512], op=ALU.add),
                           reads=[TPS[6 + hf], t_x3[j]], writes=[t_x3[j]])
                    dma(sp, x1s.ap()[ti * 128:(ti + 1) * 128, :], xt3[j][:], reads=[t_x3[j]], writes=[t_x1s], waw=False)
                def E2(j):
                    ti = g * 4 + j
                    hb_ = hn3[j % 2]; thb_ = t_hn3[j % 2]; s3_ = st3[j]; ts3_ = t_st3[j]
                    op(act, lambda: nc.scalar.activation(out=hb_[:], in_=xt3[j][:], func=AF.Square, accum_out=s3_[:, 0:1]),
                       reads=[t_x3[j]], writes=[thb_, ts3_[0]])
                    rstd_from_ss(s3_[:, 0:1], s3_[:, 2:3], D, ts3_[0], ts3_[2], s3_[:, 1:2], ts3_[1])
                    op(dve, lambda: nc.vector.scalar_tensor_tensor(out=hb_[:], in0=xt3[j][:], scalar=s3_[:, 2:3], in1=gffn_t[:],
                                                                   op0=ALU.mult, op1=ALU.mult),
                       reads=[t_x3[j], ts3_[2], t_gffn], writes=[thb_])
                    dma(sp, h2tm.ap()[ti * 128:(ti + 1) * 128, :], hb_[:], reads=[thb_], writes=[t_h2tm], waw=False)
                def E3(j):
                    ti = g * 4 + j
                    hb_ = hn3[j % 2]; thb_ = t_hn3[j % 2]; s3_ = st3[j]; ts3_ = t_st3[j]
                    transpose_gain(hb_, thb_, 128, 8, j % 2, hT3[:, :, j * 128:(j + 1) * 128], t_hT3[j], None, None)
                def E4(j):
                    ti = g * 4 + j
                    br = nbank(); brd[j] = br
                    for kc in range(8):
                        op(pe, lambda kc=kc, j=j: nc.tensor.matmul(PS[br][:, 0:36], hT3[:, kc, j * 128:(j + 1) * 128], wr[:, kc, :],
                                                                  start=(kc == 0), stop=(kc == 7)),
                           reads=[t_wr, t_hT3[j]], writes=[TPS[br]], inc=(kc == 7))
                def E5(j):
                    ti = g * 4 + j
                    br = brd[j]
                    r = rt[j % 2]; tr_ = t_rt[j % 2]
                    BIG = 1.0e4
                    op(dve, lambda: nc.vector.tensor_tensor(out=r[:, 0:36], in0=PS[br][:, 0:36], in1=brt[:], op=ALU.add),
                       reads=[TPS[br], t_brt], writes=[tr_])
                    op(dve, lambda: nc.vector.tensor_reduce(out=r[:, 136:137], in_=r[:, 0:4], axis=AX.X, op=ALU.max), reads=[tr_], writes=[tr_])
                    op(dve, lambda: nc.vector.tensor_scalar(out=r[:, 36:40], in0=r[:, 0:4], scalar1=r[:, 136:137], scalar2=None, op0=ALU.is_ge),
                       reads=[tr_], writes=[tr_])
                    op(dve, lambda: nc.vector.tensor_scalar(out=r[:, 137:138], in0=r[:, 136:137], scalar1=-1.0, scalar2=None, op0=ALU.mult),
                       reads=[tr_], writes=[tr_])
                    op(act, lambda: nc.scalar.activation(out=r[:, 150:154], in_=r[:, 0:4], func=AF.Exp, bias=r[:, 137:138], scale=1.0,
                                                         accum_out=r[:, 138:139]),
                       reads=[tr_], writes=[tr_])
                    op(dve, lambda: nc.vector.reciprocal(out=r[:, 139:140], in_=r[:, 138:139]), reads=[tr_], writes=[tr_])
                    op(dve, lambda: nc.vector.tensor_scalar(out=r[:, 36:40], in0=r[:, 36:40], scalar1=-1.0, scalar2=BIG, op0=ALU.add, op1=ALU.mult),
                       reads=[tr_], writes=[tr_])
                    op(dve, lambda: nc.vector.tensor_tensor(out=r[:, 40:72].rearrange("p (a b) -> p a b", a=4),
                                                            in0=r[:, 4:36].rearrange("p (a b) -> p a b", a=4),
                                                            in1=r[:, 36:40].unsqueeze(2).to_broadcast([128, 4, 8]), op=ALU.add),
                       reads=[tr_], writes=[tr_])
                    op(dve, lambda: nc.vector.tensor_reduce(out=r[:, 140:141], in_=r[:, 40:72], axis=AX.X, op=ALU.max), reads=[tr_], writes=[tr_])
                    op(dve, lambda: nc.vector.tensor_scalar(out=r[:, 72:104], in0=r[:, 40:72], scalar1=r[:, 140:141], scalar2=None, op0=ALU.is_ge),
                       reads=[tr_], writes=[tr_])
                    op(dve, lambda: nc.vector.scalar_tensor_tensor(out=r[:, 104:136], in0=r[:, 72:104], scalar=-BIG, in1=r[:, 40:72],
                                                                   op0=ALU.mult, op1=ALU.add),
                       reads=[tr_], writes=[tr_])
                    op(dve, lambda: nc.vector.tensor_reduce(out=r[:, 141:142], in_=r[:, 104:136], axis=AX.X, op=ALU.max), reads=[tr_], writes=[tr_])
                    op(dve, lambda: nc.vector.tensor_scalar(out=r[:, 104:136], in0=r[:, 104:136], scalar1=r[:, 141:142], scalar2=None, op0=ALU.is_ge),
                       reads=[tr_], writes=[tr_])
                    op(dve, lambda: nc.vector.tensor_tensor(out=r[:, 142:143], in0=r[:, 141:142], in1=r[:, 140:141], op=ALU.subtract),
                       reads=[tr_], writes=[tr_])
                    op(act, lambda: nc.scalar.activation(out=r[:, 143:144], in_=r[:, 142:143], func=AF.Exp), reads=[tr_], writes=[tr_])
                    op(dve, lambda: nc.vector.tensor_scalar(out=r[:, 143:144], in0=r[:, 143:144], scalar1=1.0, scalar2=None, op0=ALU.add),
                       reads=[tr_], writes=[tr_])
                    op(dve, lambda: nc.vector.reciprocal(out=r[:, 144:145], in_=r[:, 143:144]), reads=[tr_], writes=[tr_])
                    op(dve, lambda: nc.vector.tensor_scalar(out=r[:, 145:146], in0=r[:, 144:145], scalar1=-1.0, scalar2=1.0, op0=ALU.mult, op1=ALU.add),
                       reads=[tr_], writes=[tr_])
                    op(dve, lambda: nc.vector.tensor_scalar(out=r[:, 144:146], in0=r[:, 144:146], scalar1=r[:, 139:140], scalar2=None, op0=ALU.mult),
                       reads=[tr_], writes=[tr_])
                    sb_ = selb[j % 2]; tsb_ = t_selb[j % 2]
                    op(dve, lambda: nc.vector.tensor_tensor(out=sb_[:], in0=r[:, 72:104], in1=r[:, 104:136], op=ALU.add),
                       reads=[tr_], writes=[tsb_])
                    bs = nbank()
                    op(pe, lambda: nc.tensor.matmul(PS[bs][:, 0:32], umat[:], sb_[:], start=True, stop=True),
                       reads=[tsb_, t_umat], writes=[TPS[bs]], inc=False)
                    op(pe, lambda: nc.tensor.matmul(PS[bs][:, 32:64], onesm[:], sb_[:], start=True, stop=True),
                       reads=[tsb_, t_onesm], writes=[TPS[bs]])
                    op(dve, lambda: nc.vector.tensor_tensor(out=r[:, 40:72], in0=PS[bs][:, 0:32], in1=base[:], op=ALU.add),
                       reads=[TPS[bs], t_base], writes=[tr_])
                    op(dve, lambda: nc.vector.tensor_tensor(out=base[:], in0=PS[bs][:, 32:64], in1=base[:], op=ALU.add),
                       reads=[TPS[bs], t_base], writes=[t_base])
                    for k_, (c_sel, c_out) in enumerate(((72, 0), (104, 1))):
                        op(dve, lambda c_sel=c_sel: nc.vector.tensor_tensor(out=r[:, 0:32], in0=r[:, c_sel:c_sel + 32], in1=r[:, 40:72], op=ALU.mult),
                           reads=[tr_], writes=[tr_])
                        op(dve, lambda c_out=c_out, ti=ti: nc.vector.tensor_reduce(out=rinfo[:, ti, c_out:c_out + 1], in_=r[:, 0:32], axis=AX.X, op=ALU.add),
                           reads=[tr_], writes=[t_ri[ti]])
                        op(dve, lambda c_sel=c_sel: nc.vector.tensor_tensor(out=r[:, 0:32], in0=r[:, c_sel:c_sel + 32], in1=cst[:, 0:32], op=ALU.mult),
                           reads=[tr_, t_cst], writes=[tr_])
                        op(dve, lambda c_out=c_out, ti=ti: nc.vector.tensor_reduce(out=rinfo[:, ti, 2 + c_out:3 + c_out], in_=r[:, 0:32], axis=AX.X, op=ALU.add),
                           reads=[tr_], writes=[t_ri[ti]])
                    op(dve, lambda ti=ti: nc.vector.tensor_copy(out=rinfo[:, ti, 4:6], in_=r[:, 144:146]), reads=[tr_], writes=[t_ri[ti]])
                est = [E1, E2, E3, E4, E5]
                for step_ in range(ntile + len(est) - 1):
                    for si_, fn_ in enumerate(est):
                        j_ = step_ - si_
                        if 0 <= j_ < ntile:
                            fn_(j_)
            zero_fill_some(len(zfill))
            Kx.barrier()
            p3.close()
        stA.close()

        if 4 in phases:
            PoolE = mybir.EngineType.Pool
            I32 = mybir.dt.int32
            p4 = ExitStack()
            desti = sb("desti", [128, 34, 2], I32, p4); t_desti = Tr()
            bei = sb("bei", [128, NBLK + 2], I32, p4); t_bei = Tr()
            pp = ExitStack()
            big = sb("ppbig", [128, max(NBLK, NTHR, 34) * 32], F32, pp); t_big = Tr()
            sm = sb("ppsm", [128, 8, NBLK + 2], F32, pp); t_sm = Tr()
            nbl = sm[:, 0, 0:32]; cA = sm[:, 1, 0:32]; cB = sm[:, 2, 0:32]; pst = sm[:, 3, 0:32]
            op(dve, lambda: nc.vector.tensor_tensor(out=big[:, 0:32 * NTHR].rearrange("p (e j) -> p e j", j=NTHR),
                                                    in0=base[:].unsqueeze(2).to_broadcast([128, 32, NTHR]),
                                                    in1=cst[:, C_THR:C_THR + NTHR].unsqueeze(1).to_broadcast([128, 32, NTHR]), op=ALU.is_gt),
               reads=[t_base, t_cst], writes=[t_big])
            op(dve, lambda: nc.vector.tensor_reduce(out=nbl, in_=big[:, 0:32 * NTHR].rearrange("p (e j) -> p e j", j=NTHR), axis=AX.X, op=ALU.add),
               reads=[t_big], writes=[t_sm])
            op(dve, lambda: nc.vector.tensor_copy(out=cA, in_=nbl), reads=[t_sm], writes=[t_sm])
            cur, oth = cA, cB
            for d_ in (1, 2, 4, 8, 16):
                op(dve, lambda cur=cur, oth=oth: nc.vector.tensor_copy(out=oth, in_=cur), reads=[t_sm], writes=[t_sm])
                op(dve, lambda cur=cur, oth=oth, d_=d_: nc.vector.tensor_tensor(out=oth[:, d_:32], in0=cur[:, d_:32], in1=cur[:, 0:32 - d_], op=ALU.add),
                   reads=[t_sm], writes=[t_sm])
                cur, oth = oth, cur
            pend = cur
            op(dve, lambda: nc.vector.tensor_tensor(out=pst, in0=pend, in1=nbl, op=ALU.subtract), reads=[t_sm], writes=[t_sm])
            op(dve, lambda: nc.vector.tensor_tensor(out=big[:, 0:NBLK * 32].rearrange("p (b e) -> p b e", e=32),
                                                    in0=pend.unsqueeze(1).to_broadcast([128, NBLK, 32]),
                                                    in1=cst[:, C_BIO:C_BIO + NBLK].unsqueeze(2).to_broadcast([128, NBLK, 32]), op=ALU.is_le),
               reads=[t_sm, t_cst], writes=[t_big])
            op(dve, lambda: nc.vector.tensor_reduce(out=sm[:, 4, 0:NBLK], in_=big[:, 0:NBLK * 32].rearrange("p (b e) -> p b e", e=32), axis=AX.X, op=ALU.add),
               reads=[t_big], writes=[t_sm])
            op(dve, lambda: nc.vector.tensor_scalar(out=sm[:, 4, 0:NBLK - NSKIP], in0=sm[:, 4, 0:NBLK - NSKIP], scalar1=31.0, scalar2=None, op0=ALU.min),
               reads=[t_sm], writes=[t_sm])
            op(dve, lambda: nc.vector.tensor_scalar(out=sm[:, 4, 0:NBLK], in0=sm[:, 4, 0:NBLK], scalar1=128.0, scalar2=cst[:, C_PID:C_PID + 1],
                                                    op0=ALU.mult, op1=ALU.add),
               reads=[t_sm, t_cst], writes=[t_sm])
            op(dve, lambda: nc.vector.tensor_copy(out=bei[:, 0:NBLK], in_=sm[:, 4, 0:NBLK]), reads=[t_sm], writes=[t_bei])
            for k_ in range(2):
                op(dve, lambda k_=k_: nc.vector.tensor_tensor(out=big[:, 0:34 * 32].rearrange("p (t e) -> p t e", e=32),
                                                              in0=cst[:, 0:32].unsqueeze(1).to_broadcast([128, 34, 32]),
                                                              in1=rinfo[:, :, 2 + k_:3 + k_].to_broadcast([128, 34, 32]), op=ALU.is_equal),
                   reads=t_ri + [t_cst], writes=[t_big])
                op(dve, lambda: nc.vector.tensor_tensor(out=big[:, 0:34 * 32].rearrange("p (t e) -> p t e", e=32),
                                                        in0=big[:, 0:34 * 32].rearrange("p (t e) -> p t e", e=32),
                                                        in1=pst.unsqueeze(1).to_broadcast([128, 34, 32]), op=ALU.mult),
                   reads=[t_big, t_sm], writes=[t_big])
                op(dve, lambda k_=k_: nc.vector.tensor_reduce(out=sm[:, 5 + k_, 0:34], in_=big[:, 0:34 * 32].rearrange("p (t e) -> p t e", e=32),
                                                              axis=AX.X, op=ALU.add),
                   reads=[t_big], writes=[t_sm])
                op(dve, lambda k_=k_: nc.vector.scalar_tensor_tensor(out=sm[:, 5 + k_, 0:34], in0=sm[:, 5 + k_, 0:34], scalar=float(MBLK),
                                                                     in1=rinfo[:, :, k_], op0=ALU.mult, op1=ALU.add),
                   reads=t_ri + [t_sm], writes=[t_sm])
                op(dve, lambda k_=k_: nc.vector.tensor_copy(out=desti[:, :, k_], in_=sm[:, 5 + k_, 0:34]), reads=[t_sm], writes=[t_desti])
            Kx.barrier()
            pp.close()

            NW = 4
            we1 = [sb("we1_%d" % i, [128, 8, 256], BF16, p4) for i in range(NW)]; t_we1 = [Tr() for _ in range(NW)]
            we3 = [sb("we3_%d" % i, [128, 8, 256], BF16, p4) for i in range(NW)]; t_we3 = [Tr() for _ in range(NW)]
            we2 = [sb("we2_%d" % i, [128, 2, D], BF16, p4) for i in range(NW)]; t_we2 = [Tr() for _ in range(NW)]
            w1v = w1.ap().rearrange("e (p c) d -> (e p) (c d)", c=8)
            w3v = w3.ap().rearrange("e (p c) d -> (e p) (c d)", c=8)
            w2v = w2.ap().rearrange("e (p c) d -> (e p) (c d)", c=2)
            nblk_run = DBG.get("nblk", NBLK)
            NPRE = 4

            def load_w(b):
                kw = b % NW
                ix = bass.IndirectOffsetOnAxis(ap=bei[:, b:b + 1], axis=0)
                skip = dict(bounds_check=32 * 128 - 1, oob_is_err=False) if b >= NBLK - NSKIP else {}
                dma(pool, we1[kw][:].rearrange("p c d -> p (c d)"), w1v, reads=[t_bei], writes=[t_we1[kw]], indirect=(None, ix), **skip)
                dma(pool, we3[kw][:].rearrange("p c d -> p (c d)"), w3v, reads=[t_bei], writes=[t_we3[kw]], indirect=(None, ix), **skip)
                dma(pool, we2[kw][:].rearrange("p c d -> p (c d)"), w2v, reads=[t_bei], writes=[t_we2[kw]], indirect=(None, ix), **skip)

            for b_ in range(min(NPRE, nblk_run)):
                load_w(b_)

            t_hs = Tr()
            h2b = [sb("h2b%d" % i, [128, D], BF16, p4) for i in range(4)]; t_h2b = [Tr() for _ in range(4)]
            for t in range(34):
                k = t % 4
                dma(sp, h2b[k][:], h2tm.ap()[t * 128:(t + 1) * 128, :], reads=[t_h2tm], writes=[t_h2b[k]])
                for k_ in range(2):
                    dma(pool, hs.ap(), h2b[k][:], reads=[t_h2b[k], t_hsz, t_desti], writes=[t_hs], waw=False,
                        indirect=(bass.IndirectOffsetOnAxis(ap=desti[:, t, k_:k_ + 1], axis=0), None))

            wpg = sb("wpg", [128, 8, D], BF16, p4); t_wpg = Tr()
            wple = sb("wple", [128, 2, D], BF16, p4); t_wple = Tr()
            gplec, t_gplec = gaincols_load("gplec", g_ple, p4)
            hsb = [sb("hsb%d" % i, [128, MT, D], BF16, p4) for i in range(2)]; t_hsb = [Tr() for _ in range(2)]
            hsT = [sb("hsT%d" % i, [128, 8, MBLK], BF16, p4) for i in range(2)]; t_hsT = [[Tr() for _ in range(MT)] for _ in range(2)]
            sa = [sb("sa%d" % i, [128, 2 * MBLK], F32, p4) for i in range(2)]; t_sa = [Tr() for _ in range(2)]
            hid = [sb("hid%d" % i, [128, 2, MBLK], BF16, p4) for i in range(2)]; t_hid = [Tr() for _ in range(2)]
            yb = [sb("yb%d" % i, [128, D], F32, p4) for i in range(2)]; t_yb = [Tr() for _ in range(2)]
            t_ys = Tr()

            def load_block(b):
                k = b % 2
                if b >= NPRE:
                    load_w(b)
                dma(sp, hsb[k][:], hs.ap()[b * MBLK:(b + 1) * MBLK, :].rearrange("(j p) d -> p j d", p=128),
                    reads=[t_hs, t_hsz], writes=[t_hsb[k]])

            def bT(b):
                k = b % 2
                for j in range(MT):
                    transpose_gain(hsb[k][:, j, :], t_hsb[k], 128, 8, j % 2, hsT[k][:, :, j * 128:(j + 1) * 128], t_hsT[k][j], None, None, interleave=True)

            def bAB(b):
                k = b % 2
                kw = b % NW
                for hc in range(2):
                    for kc in range(8):
                        op(pe, lambda hc=hc, kc=kc: nc.tensor.matmul(PS[2][:, hc * MBLK:(hc + 1) * MBLK], we1[kw][:, kc, hc:256:2], hsT[k][:, kc, :],
                                                                    start=(kc == 0), stop=(kc == 7)),
                           reads=[t_we1[kw]] + t_hsT[k], writes=[TPS[2]], inc=(kc == 7))
                for hc in range(2):
                    for kc in range(8):
                        op(pe, lambda hc=hc, kc=kc: nc.tensor.matmul(PS[3][:, hc * MBLK:(hc + 1) * MBLK], we3[kw][:, kc, hc:256:2], hsT[k][:, kc, :],
                                                                    start=(kc == 0), stop=(kc == 7)),
                           reads=[t_we3[kw]] + t_hsT[k], writes=[TPS[3]], inc=(kc == 7))
                op(act, lambda: nc.scalar.activation(out=sa[k][:, 0:2 * MBLK], in_=PS[2][:, 0:2 * MBLK], func=AF.Silu),
                   reads=[TPS[2]], writes=[t_sa[k]])
                op(dve, lambda: nc.vector.tensor_tensor(out=hid[k][:].rearrange("p c n -> p (c n)"), in0=PS[3][:, 0:2 * MBLK],
                                                        in1=sa[k][:, 0:2 * MBLK], op=ALU.mult),
                   reads=[TPS[3], t_sa[k]], writes=[t_hid[k]])

            def bY(b):
                k = b % 2
                kw = b % NW
                for j in range(MT):
                    yk = (b * MT + j) % 2
                    for hf in range(2):
                        ob = 4 + 2 * (j % 2) + hf
                        for hc in range(2):
                            op(pe, lambda hc=hc, hf=hf, j=j, ob=ob: nc.tensor.matmul(
                                PS[ob][:, :], hid[k][:, hc, j * 128:(j + 1) * 128], we2[kw][:, hc, hf * 512:(hf + 1) * 512],
                                start=(hc == 0), stop=(hc == 1)),
                               reads=[t_hid[k], t_we2[kw]], writes=[TPS[ob]], inc=(hc == 1))
                        if hf == 0:
                            op(act, lambda ob=ob, yk=yk: nc.scalar.copy(out=yb[yk][:, 0:512], in_=PS[ob][:, :]), reads=[TPS[ob]], writes=[t_yb[yk]])
                        else:
                            op(dve, lambda ob=ob, yk=yk: nc.vector.tensor_copy(out=yb[yk][:, 512:1024], in_=PS[ob][:, :]), reads=[TPS[ob]], writes=[t_yb[yk]])
                    dma(sp, ys.ap()[b * MBLK + j * 128:b * MBLK + (j + 1) * 128, :], yb[yk][:], reads=[t_yb[yk]], writes=[t_ys], waw=False)

            if nblk_run > 0:
                load_block(0)
            for step_ in range(nblk_run + 2):
                if step_ + 1 < nblk_run:
                    load_block(step_ + 1)
                if step_ == min(6, nblk_run + 1):
                    wload(wpg, w_pg, 0, 8, D, 0, D, t_wpg)
                    wload(wple, w_ple, 0, 2, D, 0, D, t_wple)
                if step_ < nblk_run:
                    bT(step_)
                if 0 <= step_ - 1 < nblk_run:
                    bAB(step_ - 1)
                if 0 <= step_ - 2 < nblk_run:
                    bY(step_ - 2)

            NXA, NG, NP, NH4, NT4, NS4 = 5, 3, 4, 3, 3, 2
            xa = [sb("xa%d" % i, [128, D], F32, p4) for i in range(NXA)]; t_xa = [Tr() for _ in range(NXA)]
            ga = [sb("ga%d" % i, [128, D], F32, p4) for i in range(NG)]; t_ga = [Tr() for _ in range(NG)]
            gb = [sb("gb%d" % i, [128, D], F32, p4) for i in range(NG)]; t_gb = [Tr() for _ in range(NG)]
            hn4 = [sb("hn4_%d" % i, [128, D], BF16, p4) for i in range(NH4)]; t_hn4 = [Tr() for _ in range(NH4)]
            st4 = [sb("st4_%d" % i, [128, 4], F32, p4) for i in range(NH4)]; t_st4 = [[Tr() for _ in range(4)] for _ in range(NH4)]
            h3T = [sb("h3T%d" % i, [128, 8, 128], BF16, p4) for i in range(NT4)]; t_h3T = [Tr() for _ in range(NT4)]
            pb4 = [sb("pb4_%d" % i, [128, 256], BF16, p4) for i in range(NP)]; t_pb4 = [Tr() for _ in range(NP)]
            pT4 = [sb("pT4_%d" % i, [128, 2, 128], BF16, p4) for i in range(NT4)]; t_pT4 = [Tr() for _ in range(NT4)]
            sg4 = [sb("sg4_%d" % i, [128, D], F32, p4) for i in range(NS4)]; t_sg4 = [Tr() for _ in range(NS4)]
            t_yo = Tr()

            def cA(ti):
                kx, kg, kp = ti % NXA, ti % NG, ti % NP
                dma(sp, xa[kx][:], x1s.ap()[ti * 128:(ti + 1) * 128, :], reads=[t_x1s], writes=[t_xa[kx]])
                dma(pool, ga[kg][:], ys.ap(), reads=[t_ys, t_desti], writes=[t_ga[kg]],
                    indirect=(None, bass.IndirectOffsetOnAxis(ap=desti[:, ti, 0:1], axis=0)))
                dma(pool, gb[kg][:], ys.ap(), reads=[t_ys, t_desti], writes=[t_gb[kg]],
                    indirect=(None, bass.IndirectOffsetOnAxis(ap=desti[:, ti, 1:2], axis=0)))
                dma(pool, pb4[kp][:], pk.ap()[ti * 128:(ti + 1) * 128, :], writes=[t_pb4[kp]])

            def cB(ti):
                kx, kg, kh = ti % NXA, ti % NG, ti % NH4
                op(dve, lambda: nc.vector.scalar_tensor_tensor(out=xa[kx][:], in0=ga[kg][:], scalar=rinfo[:, ti, 4:5], in1=xa[kx][:],
                                                               op0=ALU.mult, op1=ALU.add),
                   reads=[t_ga[kg], t_ri[ti], t_xa[kx]], writes=[t_xa[kx]])
                op(dve, lambda: nc.vector.scalar_tensor_tensor(out=xa[kx][:], in0=gb[kg][:], scalar=rinfo[:, ti, 5:6], in1=xa[kx][:],
                                                               op0=ALU.mult, op1=ALU.add),
                   reads=[t_gb[kg], t_ri[ti], t_xa[kx]], writes=[t_xa[kx]])
                op(act, lambda: nc.scalar.activation(out=hn4[kh][:], in_=xa[kx][:], func=AF.Square, accum_out=st4[kh][:, 0:1]),
                   reads=[t_xa[kx]], writes=[t_hn4[kh], t_st4[kh][0]])
                rstd_from_ss(st4[kh][:, 0:1], st4[kh][:, 2:3], D, t_st4[kh][0], t_st4[kh][2], st4[kh][:, 1:2], t_st4[kh][1])
                op(dve, lambda: nc.vector.tensor_scalar(out=hn4[kh][:], in0=xa[kx][:], scalar1=st4[kh][:, 2:3], scalar2=None, op0=ALU.mult),
                   reads=[t_xa[kx], t_st4[kh][2]], writes=[t_hn4[kh]])

            def cC(ti):
                kh, kt, kp = ti % NH4, ti % NT4, ti % NP
                transpose_gain(hn4[kh], t_hn4[kh], 128, 8, ti % 2, h3T[kt][:], t_h3T[kt], gplec, t_gplec)
                transpose_gain(pb4[kp], t_pb4[kp], 128, 2, 2 + ti % 2, pT4[kt][:], t_pT4[kt], None, None)

            def cD(ti):
                kx, kt, ks = ti % NXA, ti % NT4, ti % NS4
                for hf in range(2):
                    ob = 4 + hf
                    for kc in range(8):
                        op(pe, lambda kc=kc, hf=hf, ob=ob: nc.tensor.matmul(PS[ob][:, :], h3T[kt][:, kc, :], wpg[:, kc, hf * 512:(hf + 1) * 512],
                                                                           start=(kc == 0), stop=(kc == 7)),
                           reads=[t_h3T[kt], t_wpg], writes=[TPS[ob]], inc=(kc == 7))
                    op(act, lambda hf=hf, ob=ob: nc.scalar.activation(out=sg4[ks][:, hf * 512:(hf + 1) * 512], in_=PS[ob][:, :], func=AF.Sigmoid),
                       reads=[TPS[ob]], writes=[t_sg4[ks]])
                    ob2 = 6 + hf
                    for kc in range(2):
                        op(pe, lambda kc=kc, hf=hf, ob2=ob2: nc.tensor.matmul(PS[ob2][:, :], pT4[kt][:, kc, :], wple[:, kc, hf * 512:(hf + 1) * 512],
                                                                             start=(kc == 0), stop=(kc == 1)),
                           reads=[t_pT4[kt], t_wple], writes=[TPS[ob2]], inc=(kc == 1))
                    op(dve, lambda hf=hf, ob2=ob2: nc.vector.tensor_tensor(out=sg4[ks][:, hf * 512:(hf + 1) * 512], in0=PS[ob2][:, :],
                                                                          in1=sg4[ks][:, hf * 512:(hf + 1) * 512], op=ALU.mult),
                       reads=[TPS[ob2], t_sg4[ks]], writes=[t_sg4[ks]])
                op(pool, lambda: nc.gpsimd.tensor_tensor(out=sg4[ks][:], in0=sg4[ks][:], in1=xa[kx][:], op=ALU.add),
                   reads=[t_sg4[ks], t_xa[kx]], writes=[t_sg4[ks]])
                dma(sp, y_o.ap()[ti * 128:(ti + 1) * 128, :], sg4[ks][:], reads=[t_sg4[ks]], writes=[t_yo], waw=False)

            cstages = [cA, cB, cC, cD]
            for step in range(34 + len(cstages) - 1):
                for si, fn in enumerate(cstages):
                    ti = step - si
                    if 0 <= ti < 34:
                        fn(ti)
            Kx.barrier()
            p4.close()

        Kx.barrier()
    return nc


_ROPE_INV = 1.0 / (10000.0 ** (np.arange(0, 32, 2, dtype=np.float32) / 32))


def _rope_tab(pos):
    ang = pos.astype(np.float32)[:, None] * _ROPE_INV[None, :]
    return np.cos(ang).astype(np.float32), np.sin(ang).astype(np.float32)


def make_core_inputs(c, inp):
    b, half = divmod(c, 2)
    own = OWN_GROUPS[half]
    oth = OWN_GROUPS[1 - half]
    order = own + oth
    xp = inp["x_prompt"][b].reshape(16, GRP, D)
    xs = inp["x_sample"][4 * c:4 * c + 4].reshape(256, D)
    xk = np.concatenate([xp[order].reshape(SEQ, D), xs], axis=0)
    pos_p = (np.array(order)[:, None] * GRP + np.arange(GRP)[None, :]).reshape(-1)
    pos_s = np.tile(1024 + np.arange(64), 4)
    pos = np.concatenate([pos_p, pos_s])
    cos, sin = _rope_tab(pos)
    tabk = np.concatenate([cos, cos, -sin, sin], axis=1).astype(np.float32)
    posq = np.concatenate([pos_p[:4096], pos_s])
    cq, sq = _rope_tab(posq)
    tabq = np.concatenate([cq.T, cq.T, -sq.T, sq.T], axis=0).astype(np.float32)
    xh = np.zeros((16, D), np.float32)
    xfull = inp["x_prompt"][b]
    for i, g in enumerate(own):
        if g > 0:
            xh[2 * i:2 * i + 2] = xfull[g * GRP - 2:g * GRP]
    maskb = np.zeros((128, 8), np.float32)
    for i in range(8):
        if N_OTHER_ACT[half][i] < N_OTHER_PROG[i]:
            maskb[:, i] = NEG
    pp = inp["p_prompt"][0, b].reshape(16, GRP, 256)[own].reshape(4096, 256)
    pk = np.concatenate([pp, inp["p_sample"][0, 4 * c:4 * c + 4].reshape(256, 256)], axis=0)
    bq = np.zeros((128, 128), np.float32)
    bq[0:64, 0:64] = 1.0 / 64
    bq[64:96, 64:128] = 1.0 / 32
    cst = np.zeros((128, CSTW), np.float32)
    cst[:, 0:32] = np.arange(32)[None, :]
    cst[:, C_THR:C_THR + NTHR] = (MBLK * np.arange(NTHR))[None, :]
    cst[:, C_BIO:C_BIO + NBLK] = np.arange(NBLK)[None, :]
    cst[:, C_PID] = np.arange(128)
    umat = np.triu(np.ones((128, 128), np.float32), k=1)
    d = {
        "umat": umat, "cst": cst,
        "xk": xk, "xh": xh,
        "latp": inp["cache_kv_latent"][0, 4 * c:4 * c + 4].reshape(4096, 256),
        "krp": inp["cache_k_rope"][0, 4 * c:4 * c + 4].reshape(4096, 32),
        "sconv": inp["state_conv"][0, 4 * c:4 * c + 4],
        "pk": pk, "tabk": tabk, "tabq": tabq, "maskb": maskb,
        "ident": np.eye(128, dtype=np.float32), "bq": bq,
    }
    for k in ("g_mix", "w_in", "g_cq", "w_uq", "g_qn", "g_qr", "g_ckv", "w_ukv", "g_kn", "g_kr", "w_oa",
              "conv_w", "w_oc", "w_o", "g_ffn", "w_rg", "b_rg", "w_re", "b_re", "w1", "w3", "w2",
              "g_ple", "w_pg", "w_ple"):
        d[k] = inp[k][0] if inp[k].ndim >= 3 or k in ("conv_w",) else inp[k]
    return {k: np.ascontiguousarray(v, dtype=np.float32) for k, v in d.items()}


PHASES = (1, 2, 3, 4)
DBG = {}


def kernel(**inputs):
    inp = {k: np.asarray(v) for k, v in inputs.items()}
    nc = build_program(PHASES)
    in_maps = [make_core_inputs(c, inp) for c in range(NCORES)]
    res = run_bass_kernel_spmd(nc, in_maps, core_ids=list(range(NCORES)))
    y_p = np.zeros((4, SEQ, D), np.float32)
    y_s = np.zeros((32, 64, D), np.float32)
    lat_p = np.zeros((1, 4, SEQ, 256), np.float32)
    kpe_p = np.zeros((1, 4, SEQ, 32), np.float32)
    conv_p = np.zeros((1, 4, 2, 512), np.float32)
    lat_s = np.zeros((1, 32, 64, 256), np.float32)
    kpe_s = np.zeros((1, 32, 64, 32), np.float32)
    conv_s = np.zeros((1, 32, 2, 512), np.float32)
    for c in range(NCORES):
        r = res.results[c]
        b, half = divmod(c, 2)
        own = OWN_GROUPS[half]
        for i, g in enumerate(own):
            sl = slice(g * GRP, (g + 1) * GRP)
            y_p[b, sl] = r["y_o"][i * GRP:(i + 1) * GRP]
            lat_p[0, b, sl] = r["lat_o"][i * GRP:(i + 1) * GRP]
            kpe_p[0, b, sl] = r["kpe_o"][i * GRP:(i + 1) * GRP]
        y_s[4 * c:4 * c + 4] = r["y_o"][4096:].reshape(4, 64, D)
        lat_s[0, 4 * c:4 * c + 4] = r["lat_o"][4096:].reshape(4, 64, 256)
        kpe_s[0, 4 * c:4 * c + 4] = r["kpe_o"][4096:].reshape(4, 64, 32)
        conv_s[0, 4 * c:4 * c + 4] = r["conv_o"][1:5]
        if half == 0:
            conv_p[0, b] = r["conv_o"][0]
    return (y_p, y_s, lat_p, kpe_p, conv_p, lat_s, kpe_s, conv_s)
```

```python
import numpy as np
from contextlib import ExitStack
import concourse.bass as bass
import concourse.mybir as mybir
from concourse.bass_utils import run_bass_kernel_spmd

F32 = mybir.dt.float32
BF16 = mybir.dt.bfloat16
AF = mybir.ActivationFunctionType
ALU = mybir.AluOpType
AX = mybir.AxisListType

D = 1024
SEQ = 8192
NCORES = 8
EPS = 1e-6
GRP = 512
OWN_GROUPS = ([0, 3, 4, 7, 8, 11, 12, 15], [1, 2, 5, 6, 9, 10, 13, 14])
N_OTHER_PROG = [1, 2, 3, 4, 5, 6, 7, 8]
N_OTHER_ACT = ([0, 2, 2, 4, 4, 6, 6, 8], [1, 1, 3, 3, 5, 5, 7, 7])
NQ = 4096 + 256
NKT = 66
SBASE = 8192
SSLOT = 1152
NK = SBASE + 4 * SSLOT
NB = NK // 128
ATTN_SCALE = 96 ** -0.5
NEG = -30000.0
MBLK = 256
NBLK = (2 * NQ) // MBLK + 32
NTHR = (2 * NQ) // MBLK
MT = MBLK // 128
C_THR = 32
C_BIO = C_THR + NTHR
C_PID = C_BIO + NBLK
CSTW = C_PID + 1
NSLOT = NBLK * MBLK
assert 2 * MBLK <= 512


class Tr:
    __slots__ = ("w", "r", "dsem", "dcnt", "name")

    def __init__(self, name=""):
        self.w = {}
        self.r = {}
        self.dsem = None
        self.dcnt = 0
        self.name = name


class Eng:
    def __init__(self, K, name, h, self_sync=True):
        self.name = name
        self.h = h
        self.sem = K.es.enter_context(K.nc.semaphore("s_" + name))
        self.cnt = 0
        self.known = {}
        self.self_sync = self_sync


class K:
    def __init__(self, nc, es):
        self.nc = nc
        self.es = es
        self.pe = Eng(self, "pe", nc.tensor, self_sync=False)
        self.dve = Eng(self, "dve", nc.vector)
        self.act = Eng(self, "act", nc.scalar)
        self.pool = Eng(self, "pool", nc.gpsimd)
        self.sp = Eng(self, "sp", nc.sync)
        self.engs = [self.pe, self.dve, self.act, self.pool, self.sp]
        self.dsems = []
        self.nsem = 0

    def _waits(self, eng, reads, writes):
        need = {}
        for t in reads:
            for k, tok in t.w.items():
                if k not in need or need[k][1] < tok[1]:
                    need[k] = tok
        for t in writes:
            for dd in (t.w, t.r):
                for k, tok in dd.items():
                    if k not in need or need[k][1] < tok[1]:
                        need[k] = tok
        for k, (sem, val) in need.items():
            if k == eng.name and not eng.self_sync:
                continue
            if eng.known.get(k, 0) >= val:
                continue
            eng.h.wait_ge(sem, val)
            eng.known[k] = val

    def op(self, eng, fn, reads=(), writes=(), inc=True):
        ex = [t for t in reads if t.name.startswith("ps")]
        if ex:
            reads = [t for t in reads if not t.name.startswith("ps")]
            writes = list(writes) + ex
        self._waits(eng, reads, writes)
        ins = fn()
        tok = (eng.sem, eng.cnt + 1)
        if inc:
            ins.then_inc(eng.sem, 1)
            eng.cnt += 1
        for t in reads:
            t.r[eng.name] = tok
        for t in writes:
            t.w[eng.name] = tok
        return ins

    def dma(self, eng, out, in_, reads=(), writes=(), waw=True, indirect=None, **kw):
        self._waits(eng, reads, writes if waw else ())
        if indirect is not None:
            ins = eng.h.indirect_dma_start(out=out, out_offset=indirect[0], in_=in_, in_offset=indirect[1], **kw)
        else:
            ins = eng.h.dma_start(out=out, in_=in_, **kw)
        t0 = writes[0] if (writes and (waw or not reads)) else reads[0]
        if t0.dsem is None:
            t0.dsem = self.es.enter_context(self.nc.semaphore("d%d" % self.nsem))
            self.nsem += 1
            self.dsems.append(t0)
        t0.dcnt += 16
        ins.then_inc(t0.dsem, 16)
        tok = (t0.dsem, t0.dcnt)
        key = "d%d" % id(t0)
        for t in reads:
            t.r[key] = tok
        for t in writes:
            t.w[key] = tok
        return ins

    def barrier(self):
        for e in self.engs:
            for o in self.engs:
                if o is e or o.cnt == 0:
                    continue
                if e.known.get(o.name, 0) < o.cnt:
                    e.h.wait_ge(o.sem, o.cnt)
                    e.known[o.name] = o.cnt
            for t in self.dsems:
                key = "d%d" % id(t)
                if e.known.get(key, 0) < t.dcnt:
                    e.h.wait_ge(t.dsem, t.dcnt)
                    e.known[key] = t.dcnt


def dap(handle, offset, pat):
    return bass.AP(handle, offset, [list(p) for p in pat])


def build_program(phases=(1, 2, 3, 4)):
    nc = bass.Bass("TRN2", target_bir_lowering=False)

    def din(name, shape, dt=F32):
        return nc.dram_tensor(name, list(shape), dt, kind="ExternalInput")

    def dout(name, shape, dt=F32):
        return nc.dram_tensor(name, list(shape), dt, kind="ExternalOutput")

    xk = din("xk", [NKT * 128, D])
    xh = din("xh", [16, D])
    latp = din("latp", [4096, 256])
    krp = din("krp", [4096, 32])
    sconv = din("sconv", [4, 2, 512])
    pk = din("pk", [NQ, 256])
    tabk = din("tabk", [NKT * 128, 64])
    tabq = din("tabq", [64, NQ])
    maskb = din("maskb", [128, 8])
    ident_d = din("ident", [128, 128])
    bq_d = din("bq", [128, 128])
    umat_d = din("umat", [128, 128])
    cst_d = din("cst", [128, CSTW])
    g_mix = din("g_mix", [1, D]); w_in = din("w_in", [D, 4128]); g_cq = din("g_cq", [1, 256])
    w_uq = din("w_uq", [256, 768]); g_qn = din("g_qn", [1, 64]); g_qr = din("g_qr", [1, 32])
    g_ckv = din("g_ckv", [1, 256]); w_ukv = din("w_ukv", [256, 1024]); g_kn = din("g_kn", [1, 64])
    g_kr = din("g_kr", [1, 32]); w_oa = din("w_oa", [512, D]); conv_w = din("conv_w", [3, 512])
    w_oc = din("w_oc", [512, D]); w_o = din("w_o", [D, D]); g_ffn = din("g_ffn", [1, D])
    w_rg = din("w_rg", [D, 4]); b_rg = din("b_rg", [1, 4]); w_re = din("w_re", [D, 32])
    b_re = din("b_re", [1, 32]); w1 = din("w1", [32, D, 256]); w3 = din("w3", [32, D, 256])
    w2 = din("w2", [32, 256, D]); g_ple = din("g_ple", [1, D]); w_pg = din("w_pg", [D, D])
    w_ple = din("w_ple", [256, D])

    y_o = dout("y_o", [NQ, D])
    lat_o = dout("lat_o", [NQ, 256])
    kpe_o = dout("kpe_o", [NQ, 32])
    conv_o = dout("conv_o", [5, 2, 512])
    attn_dbg = dout("attn_dbg", [128, 36, 512]) if DBG.get("attn_dbg", 0) else None
    x1s = nc.dram_tensor("x1s", [NQ, D], F32, kind="Internal")
    h2tm = nc.dram_tensor("h2tm", [NQ, D], BF16, kind="Internal")
    hs = nc.dram_tensor("hs", [NSLOT, D], BF16, kind="Internal")
    ys = nc.dram_tensor("ys", [NSLOT, D], F32, kind="Internal")

    es = ExitStack()
    with es:
        Kx = K(nc, es)
        pe, dve, act, pool, sp = Kx.pe, Kx.dve, Kx.act, Kx.pool, Kx.sp
        op, dma = Kx.op, Kx.dma

        def sb(name, shape, dt, stack=es):
            return stack.enter_context(nc.sbuf_tensor("sb_" + name, list(shape), dt))

        PS = [es.enter_context(nc.psum_tensor("ps%d" % i, [128, 512], F32)) for i in range(8)]
        TPS = [Tr("ps%d" % i) for i in range(8)]

        def psb(i):
            return PS[i][:].bitcast(BF16)

        ident = sb("ident", [128, 128], BF16); t_ident = Tr()
        dma(pool, ident[:], ident_d.ap(), writes=[t_ident])
        bqm = sb("bqm", [128, 128], BF16); t_bq = Tr()
        dma(pool, bqm[:], bq_d.ap(), writes=[t_bq])
        epsb = sb("epsb", [128, 1], F32); t_eps = Tr()
        op(dve, lambda: nc.vector.memset(epsb[:], EPS), writes=[t_eps])

        def bcast_load(name, src, n, stack=es):
            t = sb(name, [128, n], F32, stack)
            tr = Tr(name)
            dma(sp, t[:], dap(src, 0, [[0, 128], [1, n]]), writes=[tr])
            return t, tr

        def colvec_load(dst_ap, src, n, off, tr):
            dma(sp, dst_ap, dap(src, off, [[1, n], [1, 1]]), writes=[tr])

        def wload(dst, src, row0, kc, ncol_total, col0, ncols, tr, dst_col0=0):
            for c in range(kc):
                dma(pool, dst[:, c, dst_col0:dst_col0 + ncols],
                    dap(src, (row0 + c * 128) * ncol_total + col0, [[ncol_total, 128], [1, ncols]]),
                    writes=[tr], waw=False)

        def rstd_from_ss(ss_ap, out_ap, n, tr_in, tr_out, tmp_ap, tr_tmp):
            op(act, lambda: nc.scalar.activation(out=tmp_ap, in_=ss_ap, func=AF.Sqrt,
                                                 bias=epsb[:ss_ap.shape[0], 0:1], scale=1.0 / n),
               reads=[tr_in, t_eps], writes=[tr_tmp])
            op(dve, lambda: nc.vector.reciprocal(out=out_ap, in_=tmp_ap), reads=[tr_tmp], writes=[tr_out])

        t_x1s = Tr(); t_h2tm = Tr(); t_hsz = Tr()
        rinfo = sb("rinfo", [128, 34, 8], F32); t_ri = [Tr() for _ in range(34)]
        base = sb("base", [128, 32], F32); t_base = Tr()
        umat = sb("umat", [128, 128], BF16); t_umat = Tr()
        dma(pool, umat[:], umat_d.ap(), writes=[t_umat])
        onesm = sb("onesm", [128, 128], BF16); t_onesm = Tr()
        op(pool, lambda: nc.gpsimd.memset(onesm[:], 1.0), writes=[t_onesm])
        cst = sb("cst", [128, CSTW], F32); t_cst = Tr()
        dma(sp, cst[:], cst_d.ap(), writes=[t_cst])
        op(dve, lambda: nc.vector.memset(base[:], 0.0), writes=[t_base])
        stA = ExitStack()
        attn = sb("attn", [128, 36, 512], BF16, stA)
        t_attn = [Tr() for _ in range(36)]
        ph12 = ExitStack()
        latT = sb("latT", [128, 2, NK], BF16, ph12); t_latT = [Tr() for _ in range(NB)]
        kext = sb("kext", [128, NK], BF16, ph12); t_krope = [Tr() for _ in range(NB)]
        cqnT = sb("cqnT", [128, 2, NQ], BF16, ph12); t_cqnT = [Tr() for _ in range(NQ // 128)]

        if 1 in phases:
            p1 = ExitStack()
            gmix_t, t_gmix = bcast_load("gmix", g_mix, D, p1)
            gcq_t, t_gcq = bcast_load("gcq", g_cq, 256, p1)
            gckv_t, t_gckv = bcast_load("gckv", g_ckv, 256, p1)
            gkr_t, t_gkr = bcast_load("gkr", g_kr, 32, p1)
            wkv = sb("wkv", [128, 8, 288], BF16, p1); t_wkv = Tr()
            wload(wkv, w_in, 0, 8, 4128, 256, 288, t_wkv)
            wcq = sb("wcq", [128, 8, 256], BF16, p1); t_wcq = Tr()
            wload(wcq, w_in, 0, 8, 4128, 0, 256, t_wcq)
            NBUF = 3
            xb = [sb("xb%d" % i, [128, D], F32, p1) for i in range(NBUF)]; t_xb = [Tr() for _ in range(NBUF)]
            NTAB = 5
            tb_ = [sb("tabk%d" % i, [128, 64], F32, p1) for i in range(NTAB)]; t_tab = [Tr() for _ in range(NTAB)]
            junk = sb("junk", [128, D], BF16, p1); t_junk = Tr()
            st = [sb("st%d" % i, [128, 16], F32, p1) for i in range(NBUF)]; t_st = [[Tr() for _ in range(16)] for _ in range(NBUF)]
            hn = [sb("hn%d" % i, [128, D], BF16, p1) for i in range(2)]; t_hn = [Tr() for _ in range(2)]
            hT = [sb("hT%d" % i, [128, 8, 128], BF16, p1) for i in range(2)]; t_hT = [Tr() for _ in range(2)]
            latf = [sb("latf%d" % i, [128, 256], F32, p1) for i in range(2)]; t_latf = [Tr() for _ in range(2)]
            krn = [sb("krn%d" % i, [128, 96], F32, p1) for i in range(2)]; t_krn = [Tr() for _ in range(2)]
            kpe = [sb("kpe%d" % i, [128, 32], F32, p1) for i in range(2)]; t_kpe = [Tr() for _ in range(2)]
            tbt = [sb("tbt%d" % i, [128, 384], BF16, p1) for i in range(2)]; t_tbt = [Tr() for _ in range(2)]
            cqb = [sb("cqb%d" % i, [128, 256], BF16, p1) for i in range(2)]; t_cqb = [Tr() for _ in range(2)]
            t_lato = Tr(); t_kpeo = Tr()

            def slots_of_tile(t):
                if t < 64:
                    return [(0, 128, t * 128)]
                s0 = 2 * (t - 64)
                return [(0, 64, SBASE + s0 * SSLOT + 1024), (64, 64, SBASE + (s0 + 1) * SSLOT + 1024)]

            def is_own(t):
                return t < 32 or t >= 64

            def own_index(t):
                return t if t < 32 else t - 32

            def S1(t):
                b = t % NBUF
                dma(sp, xb[b][:], xk.ap()[t * 128:(t + 1) * 128, :], writes=[t_xb[b]])
                dma(sp, tb_[t % NTAB][:], tabk.ap()[t * 128:(t + 1) * 128, :], writes=[t_tab[t % NTAB]])
                op(act, lambda: nc.scalar.activation(out=junk[:], in_=xb[b][:], func=AF.Square,
                                                     accum_out=st[b][:, 0:1]),
                   reads=[t_xb[b]], writes=[t_junk, t_st[b][0]])
                rstd_from_ss(st[b][:, 0:1], st[b][:, 2:3], D, t_st[b][0], t_st[b][2], st[b][:, 1:2], t_st[b][1])
                h = t % 2
                op(dve, lambda: nc.vector.scalar_tensor_tensor(out=hn[h][:], in0=xb[b][:], scalar=st[b][:, 2:3],
                                                               in1=gmix_t[:], op0=ALU.mult, op1=ALU.mult),
                   reads=[t_xb[b], t_st[b][2], t_gmix], writes=[t_hn[h]])

            def S2(t):
                h = t % 2
                pb = h
                for c in range(8):
                    op(pe, lambda c=c: nc.tensor.transpose(psb(pb)[:, c * 128:(c + 1) * 128],
                                                           hn[h][:, c * 128:(c + 1) * 128], ident[:]),
                       reads=[t_hn[h], t_ident], writes=[TPS[pb]], inc=(c == 7))
                op(act, lambda: nc.scalar.copy(out=hT[h][:].rearrange("p c n -> p (c n)"), in_=psb(pb)),
                   reads=[TPS[pb]], writes=[t_hT[h]])

            def S3(t):
                h = t % 2
                pb = 2 + h
                for c in range(8):
                    op(pe, lambda c=c: nc.tensor.matmul(PS[pb][:, 0:288], hT[h][:, c, :], wkv[:, c, :],
                                                        start=(c == 0), stop=(c == 7)),
                       reads=[t_hT[h], t_wkv], writes=[TPS[pb]], inc=(c == 7))
                if is_own(t):
                    cb = 4 if h == 0 else 7
                    for c in range(8):
                        op(pe, lambda c=c: nc.tensor.matmul(PS[cb][:, 0:256], hT[h][:, c, :], wcq[:, c, :],
                                                            start=(c == 0), stop=(c == 7)),
                           reads=[t_hT[h], t_wcq], writes=[TPS[cb]], inc=(c == 7))

            def S4a(t):
                h = t % 2
                b = t % NBUF
                pb = 2 + h
                s = st[b]; ts_ = t_st[b]
                own = is_own(t)
                cb = 4 if h == 0 else 7
                op(act, lambda: nc.scalar.activation(out=junk[:, 0:256], in_=PS[pb][:, 0:256], func=AF.Square, accum_out=s[:, 3:4]),
                   reads=[TPS[pb]], writes=[t_junk, ts_[3]])
                op(act, lambda: nc.scalar.activation(out=junk[:, 256:288], in_=PS[pb][:, 256:288], func=AF.Square, accum_out=s[:, 6:7]),
                   reads=[TPS[pb]], writes=[t_junk, ts_[6]])
                if own:
                    op(act, lambda: nc.scalar.activation(out=junk[:, 512:768], in_=PS[cb][:, 0:256], func=AF.Square, accum_out=s[:, 9:10]),
                       reads=[TPS[cb]], writes=[t_junk, ts_[9]])
                rstd_from_ss(s[:, 3:4], s[:, 5:6], 256, ts_[3], ts_[5], s[:, 4:5], ts_[4])
                rstd_from_ss(s[:, 6:7], s[:, 8:9], 32, ts_[6], ts_[8], s[:, 7:8], ts_[7])
                if own:
                    rstd_from_ss(s[:, 9:10], s[:, 11:12], 256, ts_[9], ts_[11], s[:, 10:11], ts_[10])

            def S4b(t):
                h = t % 2
                b = t % NBUF
                pb = 2 + h
                s = st[b]; ts_ = t_st[b]
                kr = krn[h]
                tbl = tb_[t % NTAB]; t_tbl = t_tab[t % NTAB]
                op(dve, lambda: nc.vector.scalar_tensor_tensor(out=latf[h][:], in0=PS[pb][:, 0:256], scalar=s[:, 5:6],
                                                               in1=gckv_t[:], op0=ALU.mult, op1=ALU.mult),
                   reads=[TPS[pb], ts_[5], t_gckv], writes=[t_latf[h]])
                op(dve, lambda: nc.vector.scalar_tensor_tensor(out=kr[:, 0:32], in0=PS[pb][:, 256:288], scalar=s[:, 8:9],
                                                               in1=gkr_t[:], op0=ALU.mult, op1=ALU.mult),
                   reads=[TPS[pb], ts_[8], t_gkr], writes=[t_krn[h]])
                if is_own(t):
                    cb = 4 if h == 0 else 7
                    op(dve, lambda: nc.vector.scalar_tensor_tensor(out=cqb[h][:], in0=PS[cb][:, 0:256], scalar=s[:, 11:12],
                                                                   in1=gcq_t[:], op0=ALU.mult, op1=ALU.mult),
                       reads=[TPS[cb], ts_[11], t_gcq], writes=[t_cqb[h]])
                op(dve, lambda: nc.vector.tensor_tensor(out=kr[:, 32:64], in0=kr[:, 0:32], in1=tbl[:, 0:32], op=ALU.mult),
                   reads=[t_krn[h], t_tbl], writes=[t_krn[h]])
                op(dve, lambda: nc.vector.tensor_tensor(out=kr[:, 64:80], in0=kr[:, 16:32], in1=tbl[:, 32:48], op=ALU.mult),
                   reads=[t_krn[h], t_tbl], writes=[t_krn[h]])
                op(dve, lambda: nc.vector.tensor_tensor(out=kr[:, 80:96], in0=kr[:, 0:16], in1=tbl[:, 48:64], op=ALU.mult),
                   reads=[t_krn[h], t_tbl], writes=[t_krn[h]])
                op(dve, lambda: nc.vector.tensor_tensor(out=kpe[h][:], in0=kr[:, 32:64], in1=kr[:, 64:96], op=ALU.add),
                   reads=[t_krn[h]], writes=[t_kpe[h]])

            def S4c(t):
                h = t % 2
                op(pool, lambda: nc.gpsimd.tensor_copy(out=tbt[h][:, 0:256], in_=latf[h][:]),
                   reads=[t_latf[h]], writes=[t_tbt[h]])
                op(pool, lambda: nc.gpsimd.tensor_copy(out=tbt[h][:, 256:384].rearrange("p (a b) -> p a b", a=4),
                                                       in_=kpe[h][:].unsqueeze(1).to_broadcast([128, 4, 32])),
                   reads=[t_kpe[h]], writes=[t_tbt[h]])
                if is_own(t):
                    oi = own_index(t)
                    dma(sp, lat_o.ap()[oi * 128:(oi + 1) * 128, :], latf[h][:], reads=[t_latf[h]], writes=[t_lato], waw=False)
                    dma(sp, kpe_o.ap()[oi * 128:(oi + 1) * 128, :], kpe[h][:], reads=[t_kpe[h]], writes=[t_kpeo], waw=False)

            def tb_transposes(h, pbank, slots, nrows=128):
                for c in range(3):
                    op(pe, lambda c=c: nc.tensor.transpose(psb(pbank)[:, c * 128:c * 128 + nrows],
                                                           tbt[h][:nrows, c * 128:(c + 1) * 128], ident[:nrows, :nrows]),
                       reads=[t_tbt[h], t_ident], writes=[TPS[pbank]], inc=(c == 2))
                for (c0, n, s0) in slots:
                    blk = s0 // 128
                    op(dve, lambda c0=c0, n=n, s0=s0: nc.vector.tensor_copy(
                        out=latT[:, :, s0:s0 + n],
                        in_=psb(pbank)[:, 0:256].rearrange("p (c n) -> p c n", c=2)[:, :, c0:c0 + n]),
                       reads=[TPS[pbank]], writes=[t_latT[blk]])
                    op(act, lambda c0=c0, n=n, s0=s0: nc.scalar.copy(
                        out=kext[64:128, s0:s0 + n], in_=psb(pbank)[64:128, 256 + c0:256 + c0 + n]),
                       reads=[TPS[pbank]], writes=[t_krope[blk]])

            def S5(t):
                h = t % 2
                tb_transposes(h, 5, slots_of_tile(t))
                if is_own(t):
                    oi = own_index(t)
                    for c in range(2):
                        op(pe, lambda c=c: nc.tensor.transpose(psb(6)[:, c * 128:(c + 1) * 128],
                                                               cqb[h][:, c * 128:(c + 1) * 128], ident[:]),
                           reads=[t_cqb[h], t_ident], writes=[TPS[6]], inc=(c == 1))
                    op(dve, lambda: nc.vector.tensor_copy(
                        out=cqnT[:, :, oi * 128:(oi + 1) * 128],
                        in_=psb(6)[:, 0:256].rearrange("p (c n) -> p c n", c=2)),
                       reads=[TPS[6]], writes=[t_cqnT[oi]])

            for s_ in range(4):
                s0 = SBASE + s_ * SSLOT + 1088
                op(pool, lambda s0=s0: nc.gpsimd.memset(latT[:, :, s0:s0 + 64], 0.0), writes=[t_latT[s0 // 128]])
                op(pool, lambda s0=s0: nc.gpsimd.memset(kext[64:128, s0:s0 + 64], 0.0), writes=[t_krope[s0 // 128]])

            stages = [S1, S2, S3, S4a, S4b, S4c, S5]
            nkt = DBG.get("nkt", NKT)
            for step in range(nkt + len(stages) - 1):
                for si in reversed(range(len(stages))):
                    t = step - si
                    if 0 <= t < nkt:
                        stages[si](t)

            krpb = [sb("krpb%d" % i, [128, 32], BF16, p1) for i in range(2)]; t_krpb = [Tr() for _ in range(2)]
            for j in range(DBG.get("npast", 32)):
                h = j % 2
                s_, r = divmod(j, 8)
                dma(pool, tbt[h][:, 0:256], latp.ap()[j * 128:(j + 1) * 128, :], writes=[t_tbt[h]])
                dma(pool, krpb[h][:], krp.ap()[j * 128:(j + 1) * 128, :], writes=[t_krpb[h]])
                op(dve, lambda: nc.vector.tensor_copy(out=tbt[h][:, 256:384].rearrange("p (a b) -> p a b", a=4),
                                                      in_=krpb[h][:].unsqueeze(1).to_broadcast([128, 4, 32])),
                   reads=[t_krpb[h]], writes=[t_tbt[h]])
                tb_transposes(h, 5 + (j % 2) * 2, [(0, 128, SBASE + s_ * SSLOT + r * 128)])
            Kx.barrier()
            p1.close()

        if 2 in phases:
            p2 = ExitStack()
            wkn = sb("wkn", [128, 2, 512], BF16, p2); t_wkn = Tr()
            wv = sb("wv", [128, 2, 512], BF16, p2); t_wv = Tr()
            wq = sb("wq", [128, 2, 1024], BF16, p2); t_wq = Tr()
            for c in range(2):
                dma(pool, wkn[:, c, :].rearrange("p (h d) -> p h d", h=8),
                    dap(w_ukv, c * 128 * 1024, [[1024, 128], [128, 8], [1, 64]]), writes=[t_wkn])
                dma(pool, wv[:, c, :].rearrange("p (h d) -> p h d", h=8),
                    dap(w_ukv, c * 128 * 1024 + 64, [[1024, 128], [128, 8], [1, 64]]), writes=[t_wv])
                wq3 = wq[:, c, :].rearrange("p (h d) -> p h d", h=8)
                for (so, n, do) in ((0, 64, 0), (64, 32, 64), (80, 16, 96), (64, 16, 112)):
                    dma(pool, wq3[:, :, do:do + n],
                        dap(w_uq, c * 128 * 768 + so, [[768, 128], [96, 8], [1, n]]), writes=[t_wq])
            gq = sb("gq", [128, 1], F32, p2); t_gq = Tr()
            colvec_load(gq[0:64, :], g_qn, 64, 0, t_gq)
            colvec_load(gq[64:96, :], g_qr, 32, 0, t_gq)
            colvec_load(gq[96:112, :], g_qr, 16, 16, t_gq)
            colvec_load(gq[112:128, :], g_qr, 16, 0, t_gq)
            gkn = sb("gkn", [64, 1], F32, p2); t_gkn = Tr()
            colvec_load(gkn[:, :], g_kn, 64, 0, t_gkn)
            mkb = sb("mkb", [128, 8], F32, p2); t_mkb = Tr()
            dma(sp, mkb[:], maskb.ap(), writes=[t_mkb])
            tq = sb("tq", [128, NQ], F32, p2); t_tq = Tr()
            dma(sp, tq[64:128, :], tabq.ap(), writes=[t_tq])
            V = sb("V", [128, NB, 65], BF16, p2); t_V = [Tr() for _ in range(13)]
            for g in range(13):
                b1 = min(NB, g * 8 + 8)
                op(pool, lambda g=g, b1=b1: nc.gpsimd.memset(V[:, g * 8:b1, 64:65], 1.0), writes=[t_V[g]])
            qext = sb("qext", [128, NQ], BF16, p2); t_q = [Tr() for _ in range(9)]
            t_kn = [Tr() for _ in range(25)]
            sqb = [sb("sqb%d" % i, [128, 512], BF16, p2) for i in range(2)]; t_sqb = [Tr() for _ in range(2)]
            rsf = [sb("rsf%d" % i, [128, 512], F32, p2) for i in range(2)]; t_rsf = [Tr() for _ in range(2)]
            qtm = [sb("qtm%d" % i, [128, 512], F32, p2) for i in range(2)]; t_qtm = [Tr() for _ in range(2)]
            NSB = 4
            pT = [sb("pT%d" % i, [128, 512], BF16, p2) for i in range(NSB)]; t_pT = [Tr() for _ in range(NSB)]
            rcp = sb("rcp", [128, 8], F32, p2); t_rcp = [Tr() for _ in range(8)]

            qgroups = [(i * 512, 512) for i in range(8)] + [(4096, 256)]
            cnt_prep = [0]

            def prep_head(h):
                jobs = [("k", g, 64, 512) for g in range(25)] + [("q", gi, 128, n) for gi, (c0, n) in enumerate(qgroups)]
                nj = len(jobs)

                def J1(i):
                    kind, g, P, n = jobs[i]
                    pm = i % 5
                    for c in range(2):
                        if kind == "k":
                            op(pe, lambda c=c: nc.tensor.matmul(PS[pm][0:64, :], wkn[:, c, h * 64:(h + 1) * 64],
                                                                latT[:, c, g * 512:(g + 1) * 512], start=(c == 0), stop=(c == 1)),
                               reads=[t_wkn] + t_latT[g * 4:g * 4 + 4], writes=[TPS[pm]], inc=(c == 1))
                        else:
                            c0 = qgroups[g][0]
                            op(pe, lambda c=c: nc.tensor.matmul(PS[pm][:, :n], wq[:, c, h * 128:(h + 1) * 128],
                                                                cqnT[:, c, c0:c0 + n], start=(c == 0), stop=(c == 1)),
                               reads=[t_wq] + t_cqnT[c0 // 128:(c0 + n) // 128], writes=[TPS[pm]], inc=(c == 1))

                def J2(i):
                    kind, g, P, n = jobs[i]
                    pm = i % 5; k = i % 2
                    op(act, lambda: nc.scalar.activation(out=sqb[k][:P, :n], in_=PS[pm][:P, :n], func=AF.Square),
                       reads=[TPS[pm]], writes=[t_sqb[k]])

                def J3(i):
                    kind, g, P, n = jobs[i]
                    pst = 5 + i % 3; k = i % 2
                    bmat = bqm[0:64, 0:64] if kind == "k" else bqm[:, :]
                    op(pe, lambda: nc.tensor.matmul(PS[pst][:P, :n], bmat, sqb[k][:P, :n], start=True, stop=True),
                       reads=[t_sqb[k], t_bq], writes=[TPS[pst]])

                def J4(i):
                    kind, g, P, n = jobs[i]
                    pst = 5 + i % 3; k = i % 2
                    op(act, lambda: nc.scalar.activation(out=rsf[k][:P, :n], in_=PS[pst][:P, :n], func=AF.Ln,
                                                         bias=epsb[:P, 0:1], scale=1.0),
                       reads=[TPS[pst], t_eps], writes=[t_rsf[k]])
                    op(act, lambda: nc.scalar.activation(out=rsf[k][:P, :n], in_=rsf[k][:P, :n], func=AF.Exp, scale=-0.5),
                       reads=[t_rsf[k]], writes=[t_rsf[k]])

                def J5(i):
                    kind, g, P, n = jobs[i]
                    pm = i % 5; k = i % 2
                    if kind == "k":
                        op(dve, lambda: nc.vector.scalar_tensor_tensor(out=kext[0:64, g * 512:(g + 1) * 512], in0=PS[pm][0:64, :],
                                                                       scalar=gkn[:, 0:1], in1=rsf[k][0:64, :],
                                                                       op0=ALU.mult, op1=ALU.mult),
                           reads=[TPS[pm], t_gkn, t_rsf[k]], writes=[t_kn[g]])
                    else:
                        c0 = qgroups[g][0]
                        op(dve, lambda: nc.vector.scalar_tensor_tensor(out=qext[0:64, c0:c0 + n], in0=PS[pm][0:64, :n],
                                                                       scalar=gq[0:64, 0:1], in1=rsf[k][0:64, :n],
                                                                       op0=ALU.mult, op1=ALU.mult),
                           reads=[TPS[pm], t_gq, t_rsf[k]], writes=[t_q[g]])
                        op(dve, lambda: nc.vector.scalar_tensor_tensor(out=qtm[k][64:128, :n], in0=PS[pm][64:128, :n],
                                                                       scalar=gq[64:128, 0:1], in1=rsf[k][64:128, :n],
                                                                       op0=ALU.mult, op1=ALU.mult),
                           reads=[TPS[pm], t_gq, t_rsf[k]], writes=[t_qtm[k]])
                        op(pool, lambda: nc.gpsimd.tensor_tensor(out=qext[64:128, c0:c0 + n], in0=qtm[k][64:128, :n],
                                                                 in1=tq[64:128, c0:c0 + n], op=ALU.mult),
                           reads=[t_qtm[k], t_tq], writes=[t_q[g]])

                jst = [J1, J2, J3, J4, J5]
                for step in range(nj + len(jst) - 1):
                    for si in reversed(range(len(jst))):
                        i = step - si
                        if 0 <= i < nj:
                            jst[si](i)
                for g in range(13):
                    b0 = g * 8
                    b1 = min(NB, b0 + 8)
                    pb = g % 2
                    for bi in range(b0, b1):
                        for c in range(2):
                            op(pe, lambda bi=bi, c=c: nc.tensor.matmul(PS[pb][:, (bi - b0) * 64:(bi - b0 + 1) * 64],
                                                                       latT[:, c, bi * 128:(bi + 1) * 128],
                                                                       wv[:, c, h * 64:(h + 1) * 64], start=(c == 0), stop=(c == 1)),
                               reads=[t_wv, t_latT[bi]], writes=[TPS[pb]], inc=(c == 1 and bi == b1 - 1))
                    nb_ = b1 - b0
                    eng_, fn_ = (act, nc.scalar.copy) if g % 2 == 0 else (dve, nc.vector.tensor_copy)
                    op(eng_, lambda fn_=fn_: fn_(out=V[:, b0:b1, 0:64],
                                                 in_=PS[pb][:, 0:nb_ * 64].rearrange("p (b d) -> p b d", d=64)),
                       reads=[TPS[pb]], writes=[t_V[g]])

            def attn_tiles():
                items = []
                for i in range(8):
                    tiles = []
                    for j in range(i):
                        for r in range(4):
                            tiles.append((4 * j + r, 128, 0, 512, None, None))
                    nprog = N_OTHER_PROG[i]
                    for j in range(nprog):
                        for r in range(4):
                            tiles.append((32 + 4 * j + r, 128, 0, 512, (i if j == nprog - 1 else None), None))
                    for r in range(4):
                        tiles.append((4 * i + r, 128, r * 128, 512 - r * 128, None, r))
                    items.append((i, tiles, [(jq, 128) for jq in range(4)], [4 * i + jq for jq in range(4)]))
                for s_ in range(4):
                    kb0 = 64 + 9 * s_
                    tiles = [(kb0 + r, 128, 64 * s_, 64, None, None) for r in range(8)]
                    tiles.append((kb0 + 8, 64, 64 * s_, 64, None, None))
                    items.append((8, tiles, [(0, 64)], [32 + s_]))
                return items

            def attention_head(h):
                for (gi, tiles, qblocks, atiles) in attn_tiles():
                    qc0 = qgroups[gi][0]
                    nt = len(tiles)
                    single = (gi == 8)
                    for idx in range(nt + NSB - 1):
                        if idx < nt:
                            kb, nk, col0, ncols, bidx, diag = tiles[idx]
                            bank = idx % NSB
                            op(pe, lambda kb=kb, nk=nk, col0=col0, ncols=ncols, bank=bank: nc.tensor.matmul(
                                PS[bank][:nk, :ncols], kext[:, kb * 128:kb * 128 + nk],
                                qext[:, qc0 + col0:qc0 + col0 + ncols], start=True, stop=True),
                               reads=[t_kn[kb // 4], t_krope[kb], t_q[gi]], writes=[TPS[bank]])
                        t = idx - (NSB - 1)
                        if t < 0:
                            continue
                        kb, nk, col0, ncols, bidx, diag = tiles[t]
                        bank = t % NSB
                        bias = mkb[:nk, bidx:bidx + 1] if bidx is not None else 0.0
                        op(act, lambda nk=nk, ncols=ncols, bank=bank, bias=bias: nc.scalar.activation(
                            out=pT[bank][:nk, :ncols], in_=PS[bank][:nk, :ncols], func=AF.Exp,
                            bias=bias, scale=ATTN_SCALE),
                           reads=[TPS[bank], t_mkb], writes=[t_pT[bank]])
                        if diag is not None:
                            op(dve, lambda bank=bank: nc.vector.memset(pT[bank][64:128, 0:64], 0.0), writes=[t_pT[bank]])
                        if single:
                            qlist = [(0, 0, 64)]
                        else:
                            q_first = col0 // 128
                            qlist = [(jq, (jq - q_first) * 128, 128) for jq in range(q_first, 4)]
                        for li, (jq, off, m) in enumerate(qlist):
                            first = (t == 0)
                            last = (t == nt - 1) if single else (diag is not None and diag == jq)
                            op(pe, lambda jq=jq, off=off, m=m, nk=nk, kb=kb, bank=bank, first=first, last=last: nc.tensor.matmul(
                                PS[4 + jq][:m, 0:65], pT[bank][:nk, off:off + m], V[:nk, kb, :], start=first, stop=last),
                               reads=[t_pT[bank], t_V[kb // 8]], writes=[TPS[4 + jq]], inc=(li == len(qlist) - 1))
                    for (jq, m), at in zip(qblocks, atiles):
                        op(dve, lambda jq=jq, m=m: nc.vector.reciprocal(out=rcp[:m, jq:jq + 1], in_=PS[4 + jq][:m, 64:65]),
                           reads=[TPS[4 + jq]], writes=[t_rcp[jq]])
                        op(dve, lambda jq=jq, m=m, at=at: nc.vector.tensor_scalar(
                            out=attn[:m, at, h * 64:(h + 1) * 64], in0=PS[4 + jq][:m, 0:64],
                            scalar1=rcp[:m, jq:jq + 1], scalar2=None, op0=ALU.mult),
                           reads=[TPS[4 + jq], t_rcp[jq]], writes=[t_attn[at]])

            for h in range(DBG.get("nheads", 8)):
                if not DBG.get("noprep", 0):
                    prep_head(h)
                if not DBG.get("noattn", 0):
                    attention_head(h)
            Kx.barrier()
            if DBG.get("attn_dbg", 0):
                t_ad = Tr()
                dma(pool, attn_dbg.ap(), attn[:], reads=t_attn, writes=[t_ad])
            p2.close()

        Kx.barrier()
        ph12.close()

        def rms_tile(xt, t_x, hnb, t_hnb, stt, t_stt):
            op(act, lambda: nc.scalar.activation(out=hnb[:], in_=xt[:], func=AF.Square, accum_out=stt[:, 0:1]),
               reads=[t_x], writes=[t_hnb, t_stt[0]])
            rstd_from_ss(stt[:, 0:1], stt[:, 2:3], D, t_stt[0], t_stt[2], stt[:, 1:2], t_stt[1])
            op(dve, lambda: nc.vector.tensor_scalar(out=hnb[:], in0=xt[:], scalar1=stt[:, 2:3], scalar2=None, op0=ALU.mult),
               reads=[t_x, t_stt[2]], writes=[t_hnb])

        def transpose_gain(src, t_src, nrows, nch, pbank, dst_ap, t_dst, gcols, t_g, interleave=False):
            for c in range(nch):
                cs = slice(c, nch * 128, nch) if interleave else slice(c * 128, (c + 1) * 128)
                op(pe, lambda c=c, cs=cs: nc.tensor.transpose(psb(pbank)[:, c * 128:c * 128 + nrows],
                                                              src[:nrows, cs], ident[:nrows, :nrows]),
                   reads=[t_src, t_ident], writes=[TPS[pbank]], inc=(c == nch - 1))
            pv = psb(pbank)[:, 0:nch * 128].rearrange("p (c n) -> p c n", c=nch)[:, :, 0:nrows]
            if gcols is None:
                op(act, lambda: nc.scalar.copy(out=dst_ap, in_=pv), reads=[TPS[pbank]], writes=[t_dst])
            else:
                op(dve, lambda: nc.vector.tensor_tensor(out=dst_ap, in0=pv,
                                                        in1=gcols[:, 0:nch].unsqueeze(2).to_broadcast([128, nch, nrows]),
                                                        op=ALU.mult),
                   reads=[TPS[pbank], t_g], writes=[t_dst])

        def gaincols_load(name, src, stack):
            t = sb(name, [128, 8], F32, stack)
            tr = Tr(name)
            dma(sp, t[:], dap(src, 0, [[1, 128], [128, 8]]), writes=[tr], allow_slow_non_contiguous=True)
            return t, tr

        if 3 in phases:
            p3 = ExitStack()
            w3s = sb("w3in", [128, 8, 3584], BF16, p3)
            t_w3b = {"cx": Tr(), "cb": Tr(), "g": Tr()}

            def t_w3_of(woff):
                return t_w3b["cb"] if woff < 512 else (t_w3b["cx"] if woff < 1536 else t_w3b["g"])

            wload(w3s, w_in, 0, 8, 4128, 544 + 512, 1024, t_w3b["cx"], dst_col0=512)
            wload(w3s, w_in, 0, 8, 4128, 544, 512, t_w3b["cb"], dst_col0=0)
            wload(w3s, w_in, 0, 8, 4128, 544 + 1536, 2048, t_w3b["g"], dst_col0=1536)
            woc = sb("woc", [128, 4, D], BF16, p3); t_woc = Tr()
            wload(woc, w_oc, 0, 4, D, 0, D, t_woc)
            woa = sb("woa", [128, 4, D], BF16, p3); t_woa = Tr()
            wload(woa, w_oa, 0, 4, D, 0, D, t_woa)
            wo = sb("wo", [128, 8, D], BF16, p3); t_wo = Tr()
            wload(wo, w_o, 0, 8, D, 0, D, t_wo)
            wr = sb("wr", [128, 8, 36], BF16, p3); t_wr = Tr()
            wload(wr, w_rg, 0, 8, 4, 0, 4, t_wr)
            wload(wr, w_re, 0, 8, 32, 0, 32, t_wr, dst_col0=4)
            gmixc, t_gmixc = gaincols_load("gmixc", g_mix, p3)
            gffn_t, t_gffn = bcast_load("gffn_t", g_ffn, D, p3)
            selb = [sb("selb%d" % i, [128, 32], BF16, p3) for i in range(2)]; t_selb = [Tr() for _ in range(2)]
            zt = sb("zt", [128, 2048], BF16, p3); t_zt = Tr()
            op(pool, lambda: nc.gpsimd.memset(zt[:], 0.0), writes=[t_zt])
            hs_v = hs.ap().rearrange("(p r) d -> p (r d)", p=128)
            ZC = 2048
            zfill = [(z0, min(ZC, (NSLOT // 128) * D - z0)) for z0 in range(0, (NSLOT // 128) * D, ZC)]

            def zero_fill_some(k_):
                for _ in range(k_):
                    if zfill:
                        z0, zn = zfill.pop(0)
                        dma(sp, hs_v[:, z0:z0 + zn], zt[:, 0:zn], reads=[t_zt], writes=[t_hsz], waw=False)
            brt = sb("brt", [128, 36], F32, p3); t_brt = Tr()
            dma(sp, brt[:, 0:4], dap(b_rg, 0, [[0, 128], [1, 4]]), writes=[t_brt])
            dma(sp, brt[:, 4:36], dap(b_re, 0, [[0, 128], [1, 32]]), writes=[t_brt])
            cw = sb("cw", [128, 4, 3], F32, p3); t_cw = Tr()
            for k_ in range(3):
                dma(sp, cw[:, :, k_], dap(conv_w, k_ * 512, [[1, 128], [128, 4]]), writes=[t_cw], allow_slow_non_contiguous=True)
            xt3 = [sb("x3_%d" % i, [128, D], F32, p3) for i in range(4)]; t_x3 = [Tr() for _ in range(4)]
            hn3 = [sb("hn3_%d" % i, [128, D], BF16, p3) for i in range(2)]; t_hn3 = [Tr() for _ in range(2)]
            st3 = [sb("st3_%d" % i, [128, 4], F32, p3) for i in range(4)]; t_st3 = [[Tr() for _ in range(4)] for _ in range(4)]
            hT3 = sb("hT3", [128, 8, 512], BF16, p3); t_hT3 = [Tr() for _ in range(4)]
            upad = sb("upad", [128, 4, 516], F32, p3); t_up = [Tr() for _ in range(4)]
            uhalo = sb("uhalo", [128, 4, 16], F32, p3); t_uh = Tr()
            cc = sb("cc", [128, 512], F32, p3); t_cc = Tr()
            cv = sb("cv", [128, 512], F32, p3); t_cv = Tr()
            bc = sb("bc", [128, 4, 512], BF16, p3); t_bc = [Tr() for _ in range(4)]
            tm = [sb("tm%d" % i, [128, 512], F32, p3) for i in range(2)]; t_tm = [Tr() for _ in range(2)]
            mT = sb("mT", [128, 8, 512], BF16, p3); t_mT = [Tr() for _ in range(8)]
            aT = sb("aT", [128, 4, 512], BF16, p3); t_aT = [Tr() for _ in range(4)]
            rt = [sb("rt%d" % i, [128, 160], F32, p3) for i in range(2)]; t_rt = [Tr() for _ in range(2)]
            t_convo = Tr()
            bank_rr = [0]

            def nbank():
                b = 2 + (bank_rr[0] % 4)
                bank_rr[0] += 1
                return b

            def fm_matmul(bank, wt, t_w, woff, rhs_of_kc, nk, n, reads):
                for kc in range(nk):
                    op(pe, lambda kc=kc: nc.tensor.matmul(PS[bank][:, :n], wt[:, kc, woff:woff + 128], rhs_of_kc(kc),
                                                          start=(kc == 0), stop=(kc == nk - 1)),
                       reads=[t_w] + reads, writes=[TPS[bank]], inc=(kc == nk - 1))

            dma(sp, xt3[0][0:16, :], xh.ap(), writes=[t_x3[0]])
            op(act, lambda: nc.scalar.activation(out=hn3[0][0:16, :], in_=xt3[0][0:16, :], func=AF.Square,
                                                 accum_out=st3[0][0:16, 0:1]),
               reads=[t_x3[0]], writes=[t_hn3[0], t_st3[0][0]])
            rstd_from_ss(st3[0][0:16, 0:1], st3[0][0:16, 2:3], D, t_st3[0][0], t_st3[0][2], st3[0][0:16, 1:2], t_st3[0][1])
            op(dve, lambda: nc.vector.tensor_scalar(out=hn3[0][0:16, :], in0=xt3[0][0:16, :], scalar1=st3[0][0:16, 2:3],
                                                    scalar2=None, op0=ALU.mult),
               reads=[t_x3[0], t_st3[0][2]], writes=[t_hn3[0]])
            transpose_gain(hn3[0], t_hn3[0], 16, 8, 0, hT3[:, :, 0:16], t_hT3[0], gmixc, t_gmixc)
            for c in range(4):
                b1 = nbank()
                fm_matmul(b1, w3s, t_w3_of(512), 512 + c * 128, lambda kc: hT3[:, kc, 0:16], 8, 16, [t_hT3[0]])
                op(act, lambda b1=b1: nc.scalar.copy(out=cc[:, 0:16], in_=PS[b1][:, 0:16]), reads=[TPS[b1]], writes=[t_cc])
                b2 = nbank()
                fm_matmul(b2, w3s, t_w3_of(1024), 1024 + c * 128, lambda kc: hT3[:, kc, 0:16], 8, 16, [t_hT3[0]])
                op(dve, lambda c=c, b2=b2: nc.vector.tensor_tensor(out=uhalo[:, c, :], in0=PS[b2][:, 0:16], in1=cc[:, 0:16], op=ALU.mult),
                   reads=[TPS[b2], t_cc], writes=[t_uh])

            n_groups3 = DBG.get("ngroups3", 9)
            for g in range(n_groups3):
                samp = (g == 8)
                ntile = 2 if samp else 4
                n = ntile * 128
                q0 = g * 512
                nseq, L = (4, 64) if samp else (1, 512)
                up4 = upad[:, :, 0:nseq * (L + 2)].rearrange("p c (s l) -> p c s l", s=nseq)
                for j in range(ntile):
                    dma(sp, xt3[j][:], xk.ap()[(64 * 128 + j * 128 if samp else q0 + j * 128):(64 * 128 + j * 128 if samp else q0 + j * 128) + 128, :],
                        writes=[t_x3[j]])
                    rms_tile(xt3[j], t_x3[j], hn3[j % 2], t_hn3[j % 2], st3[j], t_st3[j])
                    transpose_gain(hn3[j % 2], t_hn3[j % 2], 128, 8, j % 2, hT3[:, :, j * 128:(j + 1) * 128], t_hT3[j], gmixc, t_gmixc)
                hreads = t_hT3[0:ntile]
                if samp:
                    for s_ in range(4):
                        transpose_gain(attn[:, 32 + s_, :], t_attn[32 + s_], 64, 4, s_ % 2, aT[:, :, s_ * 64:(s_ + 1) * 64], t_aT[s_], None, None)
                else:
                    for j in range(4):
                        transpose_gain(attn[:, g * 4 + j, :], t_attn[g * 4 + j], 128, 4, j % 2, aT[:, :, j * 128:(j + 1) * 128], t_aT[j], None, None)
                for c in range(4):
                    if samp:
                        for k_ in range(2):
                            dma(sp, up4[:, c, :, k_], dap(sconv, c * 128 + k_ * 512, [[1, 128], [1024, 4]]),
                                writes=[t_up[c]], allow_slow_non_contiguous=True)
                    else:
                        op(pool, lambda c=c: nc.gpsimd.tensor_copy(out=upad[:, c, 0:2], in_=uhalo[:, c, 2 * g:2 * g + 2]),
                           reads=[t_uh], writes=[t_up[c]])
                    b1 = nbank()
                    fm_matmul(b1, w3s, t_w3_of(512), 512 + c * 128, lambda kc: hT3[:, kc, 0:n], 8, n, hreads)
                    op(act, lambda b1=b1: nc.scalar.copy(out=cc[:, 0:n], in_=PS[b1][:, 0:n]), reads=[TPS[b1]], writes=[t_cc])
                    b2 = nbank()
                    fm_matmul(b2, w3s, t_w3_of(1024), 1024 + c * 128, lambda kc: hT3[:, kc, 0:n], 8, n, hreads)
                    op(dve, lambda c=c, b2=b2: nc.vector.tensor_tensor(
                        out=up4[:, c, :, 2:2 + L], in0=PS[b2][:, 0:n].rearrange("p (s l) -> p s l", s=nseq),
                        in1=cc[:, 0:n].rearrange("p (s l) -> p s l", s=nseq), op=ALU.mult),
                       reads=[TPS[b2], t_cc], writes=[t_up[c]])
                    cv3 = cv[:, 0:n].rearrange("p (s l) -> p s l", s=nseq)
                    op(dve, lambda c=c: nc.vector.tensor_scalar(out=cv3, in0=up4[:, c, :, 0:L], scalar1=cw[:, c, 0:1],
                                                                scalar2=None, op0=ALU.mult),
                       reads=[t_up[c], t_cw], writes=[t_cv])
                    for k_ in (1, 2):
                        op(dve, lambda c=c, k_=k_: nc.vector.scalar_tensor_tensor(
                            out=cv3, in0=up4[:, c, :, k_:k_ + L], scalar=cw[:, c, k_:k_ + 1], in1=cv3,
                            op0=ALU.mult, op1=ALU.add),
                           reads=[t_up[c], t_cw, t_cv], writes=[t_cv])
                    b3 = nbank()
                    fm_matmul(b3, w3s, t_w3_of(0), 0 + c * 128, lambda kc: hT3[:, kc, 0:n], 8, n, hreads)
                    op(dve, lambda c=c, b3=b3: nc.vector.tensor_tensor(out=bc[:, c, 0:n], in0=PS[b3][:, 0:n], in1=cv[:, 0:n], op=ALU.mult),
                       reads=[TPS[b3], t_cv], writes=[t_bc[c]])
                    if samp:
                        for k_ in range(2):
                            dma(sp, dap(conv_o, 1024 + c * 128 + k_ * 512, [[1, 128], [1024, 4]]), up4[:, c, :, L + k_],
                                reads=[t_up[c]], writes=[t_convo], allow_slow_non_contiguous=True)
                    elif g == 7:
                        dma(sp, dap(conv_o, c * 128, [[1, 128], [512, 2]]), upad[:, c, L:L + 2],
                            reads=[t_up[c]], writes=[t_convo], allow_slow_non_contiguous=True)
                for c in range(8):
                    bg = nbank()
                    fm_matmul(bg, w3s, t_w3_of(2560), 2560 + c * 128, lambda kc: hT3[:, kc, 0:n], 8, n, hreads)
                    op(act, lambda bg=bg: nc.scalar.activation(out=tm[0][:, 0:n], in_=PS[bg][:, 0:n], func=AF.Sigmoid),
                       reads=[TPS[bg]], writes=[t_tm[0]])
                    by = nbank()
                    fm_matmul(by, woc, t_woc, c * 128, lambda kc: bc[:, kc, 0:n], 4, n, t_bc)
                    op(dve, lambda by=by: nc.vector.tensor_tensor(out=tm[0][:, 0:n], in0=PS[by][:, 0:n], in1=tm[0][:, 0:n], op=ALU.mult),
                       reads=[TPS[by], t_tm[0]], writes=[t_tm[0]])
                    bg2 = nbank()
                    fm_matmul(bg2, w3s, t_w3_of(1536), 1536 + c * 128, lambda kc: hT3[:, kc, 0:n], 8, n, hreads)
                    op(act, lambda bg2=bg2: nc.scalar.activation(out=tm[1][:, 0:n], in_=PS[bg2][:, 0:n], func=AF.Sigmoid),
                       reads=[TPS[bg2]], writes=[t_tm[1]])
                    by2 = nbank()
                    fm_matmul(by2, woa, t_woa, c * 128, lambda kc: aT[:, kc, 0:n], 4, n, t_aT)
                    op(dve, lambda by2=by2: nc.vector.tensor_tensor(out=tm[1][:, 0:n], in0=PS[by2][:, 0:n], in1=tm[1][:, 0:n], op=ALU.mult),
                       reads=[TPS[by2], t_tm[1]], writes=[t_tm[1]])
                    op(pool, lambda c=c: nc.gpsimd.tensor_tensor(out=mT[:, c, 0:n], in0=tm[0][:, 0:n], in1=tm[1][:, 0:n], op=ALU.add),
                       reads=[t_tm[0], t_tm[1]], writes=[t_mT[c]])
                zero_fill_some((len(zfill) + (n_groups3 - g) - 1) // (n_groups3 - g))
                brd = {}
                def E1(j):
                    ti = g * 4 + j
                    ti = g * 4 + j
                    for hf in range(2):
                        for kc in range(8):
                            op(pe, lambda kc=kc, hf=hf, j=j: nc.tensor.matmul(PS[6 + hf][:, :], mT[:, kc, j * 128:(j + 1) * 128],
                                                                            wo[:, kc, hf * 512:(hf + 1) * 512],
                                                                            start=(kc == 0), stop=(kc == 7)),
                               reads=[t_wo] + t_mT, writes=[TPS[6 + hf]], inc=(kc == 7))
                        op(dve, lambda hf=hf, j=j: nc.vector.tensor_tensor(out=xt3[j][:, hf * 512:(hf + 1) * 512], in0=PS[6 + hf][:, :],
                                                                          in1=xt3[j][:, hf * 512:(hf + 1) * 512], op=ALU.add),
                           reads=[TPS[6 + hf], t_x3[j]], writes=[t_x3[j]])
                    dma(sp, x1s.ap()[ti * 128:(ti + 1) * 128, :], xt3[j][:], reads=[t_x3[j]], writes=[t_x1s], waw=False)
                def E2(j):
                    ti = g * 4 + j
                    hb_ = hn3[j % 2]; thb_ = t_hn3[j % 2]; s3_ = st3[j]; ts3_ = t_st3[j]
                    op(act, lambda: nc.scalar.activation(out=hb_[:], in_=xt3[j][:], func=AF.Square, accum_out=s3_[:, 0:1]),
                       reads=[t_x3[j]], writes=[thb_, ts3_[0]])
                    rstd_from_ss(s3_[:, 0:1], s3_[:, 2:3], D, ts3_[0], ts3_[2], s3_[:, 1:2], ts3_[1])
                    op(dve, lambda: nc.vector.scalar_tensor_tensor(out=hb_[:], in0=xt3[j][:], scalar=s3_[:, 2:3], in1=gffn_t[:],
                                                                   op0=ALU.mult, op1=ALU.mult),
                       reads=[t_x3[j], ts3_[2], t_gffn], writes=[thb_])
                    dma(sp, h2tm.ap()[ti * 128:(ti + 1) * 128, :], hb_[:], reads=[thb_], writes=[t_h2tm], waw=False)
                def E3(j):
                    ti = g * 4 + j
                    hb_ = hn3[j % 2]; thb_ = t_hn3[j % 2]; s3_ = st3[j]; ts3_ = t_st3[j]
                    transpose_gain(hb_, thb_, 128, 8, j % 2, hT3[:, :, j * 128:(j + 1) * 128], t_hT3[j], None, None)
                def E4(j):
                    ti = g * 4 + j
                    br = nbank(); brd[j] = br
                    for kc in range(8):
                        op(pe, lambda kc=kc, j=j: nc.tensor.matmul(PS[br][:, 0:36], hT3[:, kc, j * 128:(j + 1) * 128], wr[:, kc, :],
                                                                  start=(kc == 0), stop=(kc == 7)),
                           reads=[t_wr, t_hT3[j]], writes=[TPS[br]], inc=(kc == 7))
                def E5(j):
                    ti = g * 4 + j
                    br = brd[j]
                    r = rt[j % 2]; tr_ = t_rt[j % 2]
                    BIG = 1.0e4
                    op(dve, lambda: nc.vector.tensor_tensor(out=r[:, 0:36], in0=PS[br][:, 0:36], in1=brt[:], op=ALU.add),
                       reads=[TPS[br], t_brt], writes=[tr_])
                    op(dve, lambda: nc.vector.tensor_reduce(out=r[:, 136:137], in_=r[:, 0:4], axis=AX.X, op=ALU.max), reads=[tr_], writes=[tr_])
                    op(dve, lambda: nc.vector.tensor_scalar(out=r[:, 36:40], in0=r[:, 0:4], scalar1=r[:, 136:137], scalar2=None, op0=ALU.is_ge),
                       reads=[tr_], writes=[tr_])
                    op(dve, lambda: nc.vector.tensor_scalar(out=r[:, 137:138], in0=r[:, 136:137], scalar1=-1.0, scalar2=None, op0=ALU.mult),
                       reads=[tr_], writes=[tr_])
                    op(act, lambda: nc.scalar.activation(out=r[:, 150:154], in_=r[:, 0:4], func=AF.Exp, bias=r[:, 137:138], scale=1.0,
                                                         accum_out=r[:, 138:139]),
                       reads=[tr_], writes=[tr_])
                    op(dve, lambda: nc.vector.reciprocal(out=r[:, 139:140], in_=r[:, 138:139]), reads=[tr_], writes=[tr_])
                    op(dve, lambda: nc.vector.tensor_scalar(out=r[:, 36:40], in0=r[:, 36:40], scalar1=-1.0, scalar2=BIG, op0=ALU.add, op1=ALU.mult),
                       reads=[tr_], writes=[tr_])
                    op(dve, lambda: nc.vector.tensor_tensor(out=r[:, 40:72].rearrange("p (a b) -> p a b", a=4),
                                                            in0=r[:, 4:36].rearrange("p (a b) -> p a b", a=4),
                                                            in1=r[:, 36:40].unsqueeze(2).to_broadcast([128, 4, 8]), op=ALU.add),
                       reads=[tr_], writes=[tr_])
                    op(dve, lambda: nc.vector.tensor_reduce(out=r[:, 140:141], in_=r[:, 40:72], axis=AX.X, op=ALU.max), reads=[tr_], writes=[tr_])
                    op(dve, lambda: nc.vector.tensor_scalar(out=r[:, 72:104], in0=r[:, 40:72], scalar1=r[:, 140:141], scalar2=None, op0=ALU.is_ge),
                       reads=[tr_], writes=[tr_])
                    op(dve, lambda: nc.vector.scalar_tensor_tensor(out=r[:, 104:136], in0=r[:, 72:104], scalar=-BIG, in1=r[:, 40:72],
                                                                   op0=ALU.mult, op1=ALU.add),
                       reads=[tr_], writes=[tr_])
                    op(dve, lambda: nc.vector.tensor_reduce(out=r[:, 141:142], in_=r[:, 104:136], axis=AX.X, op=ALU.max), reads=[tr_], writes=[tr_])
                    op(dve, lambda: nc.vector.tensor_scalar(out=r[:, 104:136], in0=r[:, 104:136], scalar1=r[:, 141:142], scalar2=None, op0=ALU.is_ge),
                       reads=[tr_], writes=[tr_])
                    op(dve, lambda: nc.vector.tensor_tensor(out=r[:, 142:143], in0=r[:, 141:142], in1=r[:, 140:141], op=ALU.subtract),
                       reads=[tr_], writes=[tr_])
                    op(act, lambda: nc.scalar.activation(out=r[:, 143:144], in_=r[:, 142:143], func=AF.Exp), reads=[tr_], writes=[tr_])
                    op(dve, lambda: nc.vector.tensor_scalar(out=r[:, 143:144], in0=r[:, 143:144], scalar1=1.0, scalar2=None, op0=ALU.add),
                       reads=[tr_], writes=[tr_])
                    op(dve, lambda: nc.vector.reciprocal(out=r[:, 144:145], in_=r[:, 143:144]), reads=[tr_], writes=[tr_])
                    op(dve, lambda: nc.vector.tensor_scalar(out=r[:, 145:146], in0=r[:, 144:145], scalar1=-1.0, scalar2=1.0, op0=ALU.mult, op1=ALU.add),
                       reads=[tr_], writes=[tr_])
                    op(dve, lambda: nc.vector.tensor_scalar(out=r[:, 144:146], in0=r[:, 144:146], scalar1=r[:, 139:140], scalar2=None, op0=ALU.mult),
                       reads=[tr_], writes=[tr_])
                    sb_ = selb[j % 2]; tsb_ = t_selb[j % 2]
                    op(dve, lambda: nc.vector.tensor_tensor(out=sb_[:], in0=r[:, 72:104], in1=r[:, 104:136], op=ALU.add),
                       reads=[tr_], writes=[tsb_])
                    bs = nbank()
                    op(pe, lambda: nc.tensor.matmul(PS[bs][:, 0:32], umat[:], sb_[:], start=True, stop=True),
                       reads=[tsb_, t_umat], writes=[TPS[bs]], inc=False)
                    op(pe, lambda: nc.tensor.matmul(PS[bs][:, 32:64], onesm[:], sb_[:], start=True, stop=True),
                       reads=[tsb_, t_onesm], writes=[TPS[bs]])
                    op(dve, lambda: nc.vector.tensor_tensor(out=r[:, 40:72], in0=PS[bs][:, 0:32], in1=base[:], op=ALU.add),
                       reads=[TPS[bs], t_base], writes=[tr_])
                    op(dve, lambda: nc.vector.tensor_tensor(out=base[:], in0=PS[bs][:, 32:64], in1=base[:], op=ALU.add),
                       reads=[TPS[bs], t_base], writes=[t_base])
                    for k_, (c_sel, c_out) in enumerate(((72, 0), (104, 1))):
                        op(dve, lambda c_sel=c_sel: nc.vector.tensor_tensor(out=r[:, 0:32], in0=r[:, c_sel:c_sel + 32], in1=r[:, 40:72], op=ALU.mult),
                           reads=[tr_], writes=[tr_])
                        op(dve, lambda c_out=c_out, ti=ti: nc.vector.tensor_reduce(out=rinfo[:, ti, c_out:c_out + 1], in_=r[:, 0:32], axis=AX.X, op=ALU.add),
                           reads=[tr_], writes=[t_ri[ti]])
                        op(dve, lambda c_sel=c_sel: nc.vector.tensor_tensor(out=r[:, 0:32], in0=r[:, c_sel:c_sel + 32], in1=cst[:, 0:32], op=ALU.mult),
                           reads=[tr_, t_cst], writes=[tr_])
                        op(dve, lambda c_out=c_out, ti=ti: nc.vector.tensor_reduce(out=rinfo[:, ti, 2 + c_out:3 + c_out], in_=r[:, 0:32], axis=AX.X, op=ALU.add),
                           reads=[tr_], writes=[t_ri[ti]])
                    op(dve, lambda ti=ti: nc.vector.tensor_copy(out=rinfo[:, ti, 4:6], in_=r[:, 144:146]), reads=[tr_], writes=[t_ri[ti]])
                est = [E1, E2, E3, E4, E5]
                for step_ in range(ntile + len(est) - 1):
                    for si_, fn_ in enumerate(est):
                        j_ = step_ - si_
                        if 0 <= j_ < ntile:
                            fn_(j_)
            zero_fill_some(len(zfill))
            Kx.barrier()
            p3.close()
        stA.close()

        if 4 in phases:
            PoolE = mybir.EngineType.Pool
            I32 = mybir.dt.int32
            p4 = ExitStack()
            desti = sb("desti", [128, 34, 2], I32, p4); t_desti = Tr()
            bei = sb("bei", [128, NBLK + 2], I32, p4); t_bei = Tr()
            pp = ExitStack()
            big = sb("ppbig", [128, max(NBLK, NTHR, 34) * 32], F32, pp); t_big = Tr()
            sm = sb("ppsm", [128, 8, NBLK + 2], F32, pp); t_sm = Tr()
            nbl = sm[:, 0, 0:32]; cA = sm[:, 1, 0:32]; cB = sm[:, 2, 0:32]; pst = sm[:, 3, 0:32]
            op(dve, lambda: nc.vector.tensor_tensor(out=big[:, 0:32 * NTHR].rearrange("p (e j) -> p e j", j=NTHR),
                                                    in0=base[:].unsqueeze(2).to_broadcast([128, 32, NTHR]),
                                                    in1=cst[:, C_THR:C_THR + NTHR].unsqueeze(1).to_broadcast([128, 32, NTHR]), op=ALU.is_gt),
               reads=[t_base, t_cst], writes=[t_big])
            op(dve, lambda: nc.vector.tensor_reduce(out=nbl, in_=big[:, 0:32 * NTHR].rearrange("p (e j) -> p e j", j=NTHR), axis=AX.X, op=ALU.add),
               reads=[t_big], writes=[t_sm])
            op(dve, lambda: nc.vector.tensor_copy(out=cA, in_=nbl), reads=[t_sm], writes=[t_sm])
            cur, oth = cA, cB
            for d_ in (1, 2, 4, 8, 16):
                op(dve, lambda cur=cur, oth=oth: nc.vector.tensor_copy(out=oth, in_=cur), reads=[t_sm], writes=[t_sm])
                op(dve, lambda cur=cur, oth=oth, d_=d_: nc.vector.tensor_tensor(out=oth[:, d_:32], in0=cur[:, d_:32], in1=cur[:, 0:32 - d_], op=ALU.add),
                   reads=[t_sm], writes=[t_sm])
                cur, oth = oth, cur
            pend = cur
            op(dve, lambda: nc.vector.tensor_tensor(out=pst, in0=pend, in1=nbl, op=ALU.subtract), reads=[t_sm], writes=[t_sm])
            op(dve, lambda: nc.vector.tensor_tensor(out=big[:, 0:NBLK * 32].rearrange("p (b e) -> p b e", e=32),
                                                    in0=pend.unsqueeze(1).to_broadcast([128, NBLK, 32]),
                                                    in1=cst[:, C_BIO:C_BIO + NBLK].unsqueeze(2).to_broadcast([128, NBLK, 32]), op=ALU.is_le),
               reads=[t_sm, t_cst], writes=[t_big])
            op(dve, lambda: nc.vector.tensor_reduce(out=sm[:, 4, 0:NBLK], in_=big[:, 0:NBLK * 32].rearrange("p (b e) -> p b e", e=32), axis=AX.X, op=ALU.add),
               reads=[t_big], writes=[t_sm])
            op(dve, lambda: nc.vector.tensor_scalar(out=sm[:, 4, 0:NBLK], in0=sm[:, 4, 0:NBLK], scalar1=31.0, scalar2=None, op0=ALU.min),
               reads=[t_sm], writes=[t_sm])
            op(dve, lambda: nc.vector.tensor_scalar(out=sm[:, 4, 0:NBLK], in0=sm[:, 4, 0:NBLK], scalar1=128.0, scalar2=cst[:, C_PID:C_PID + 1],
                                                    op0=ALU.mult, op1=ALU.add),
               reads=[t_sm, t_cst], writes=[t_sm])
            op(dve, lambda: nc.vector.tensor_copy(out=bei[:, 0:NBLK], in_=sm[:, 4, 0:NBLK]), reads=[t_sm], writes=[t_bei])
            for k_ in range(2):
                op(dve, lambda k_=k_: nc.vector.tensor_tensor(out=big[:, 0:34 * 32].rearrange("p (t e) -> p t e", e=32),
                                                              in0=cst[:, 0:32].unsqueeze(1).to_broadcast([128, 34, 32]),
                                                              in1=rinfo[:, :, 2 + k_:3 + k_].to_broadcast([128, 34, 32]), op=ALU.is_equal),
                   reads=t_ri + [t_cst], writes=[t_big])
                op(dve, lambda: nc.vector.tensor_tensor(out=big[:, 0:34 * 32].rearrange("p (t e) -> p t e", e=32),
                                                        in0=big[:, 0:34 * 32].rearrange("p (t e) -> p t e", e=32),
                                                        in1=pst.unsqueeze(1).to_broadcast([128, 34, 32]), op=ALU.mult),
                   reads=[t_big, t_sm], writes=[t_big])
                op(dve, lambda k_=k_: nc.vector.tensor_reduce(out=sm[:, 5 + k_, 0:34], in_=big[:, 0:34 * 32].rearrange("p (t e) -> p t e", e=32),
                                                              axis=AX.X, op=ALU.add),
                   reads=[t_big], writes=[t_sm])
                op(dve, lambda k_=k_: nc.vector.scalar_tensor_tensor(out=sm[:, 5 + k_, 0:34], in0=sm[:, 5 + k_, 0:34], scalar=float(MBLK),
                                                                     in1=rinfo[:, :, k_], op0=ALU.mult, op1=ALU.add),
                   reads=t_ri + [t_sm], writes=[t_sm])
                op(dve, lambda k_=k_: nc.vector.tensor_copy(out=desti[:, :, k_], in_=sm[:, 5 + k_, 0:34]), reads=[t_sm], writes=[t_desti])
            Kx.barrier()
            pp.close()

            NW = 4
            we1 = [sb("we1_%d" % i, [128, 8, 256], BF16, p4) for i in range(NW)]; t_we1 = [Tr() for _ in range(NW)]
            we3 = [sb("we3_%d" % i, [128, 8, 256], BF16, p4) for i in range(NW)]; t_we3 = [Tr() for _ in range(NW)]
            we2 = [sb("we2_%d" % i, [128, 2, D], BF16, p4) for i in range(NW)]; t_we2 = [Tr() for _ in range(NW)]
            w1v = w1.ap().rearrange("e (p c) d -> (e p) (c d)", c=8)
            w3v = w3.ap().rearrange("e (p c) d -> (e p) (c d)", c=8)
            w2v = w2.ap().rearrange("e (p c) d -> (e p) (c d)", c=2)
            nblk_run = DBG.get("nblk", NBLK)
            NPRE = 4

            def load_w(b):
                kw = b % NW
                ix = bass.IndirectOffsetOnAxis(ap=bei[:, b:b + 1], axis=0)
                dma(pool, we1[kw][:].rearrange("p c d -> p (c d)"), w1v, reads=[t_bei], writes=[t_we1[kw]], indirect=(None, ix))
                dma(pool, we3[kw][:].rearrange("p c d -> p (c d)"), w3v, reads=[t_bei], writes=[t_we3[kw]], indirect=(None, ix))
                dma(pool, we2[kw][:].rearrange("p c d -> p (c d)"), w2v, reads=[t_bei], writes=[t_we2[kw]], indirect=(None, ix))

            for b_ in range(min(NPRE, nblk_run)):
                load_w(b_)

            t_hs = Tr()
            h2b = [sb("h2b%d" % i, [128, D], BF16, p4) for i in range(4)]; t_h2b = [Tr() for _ in range(4)]
            for t in range(34):
                k = t % 4
                dma(sp, h2b[k][:], h2tm.ap()[t * 128:(t + 1) * 128, :], reads=[t_h2tm], writes=[t_h2b[k]])
                for k_ in range(2):
                    dma(pool, hs.ap(), h2b[k][:], reads=[t_h2b[k], t_hsz, t_desti], writes=[t_hs], waw=False,
                        indirect=(bass.IndirectOffsetOnAxis(ap=desti[:, t, k_:k_ + 1], axis=0), None))

            wpg = sb("wpg", [128, 8, D], BF16, p4); t_wpg = Tr()
            wple = sb("wple", [128, 2, D], BF16, p4); t_wple = Tr()
            gplec, t_gplec = gaincols_load("gplec", g_ple, p4)
            hsb = [sb("hsb%d" % i, [128, MT, D], BF16, p4) for i in range(2)]; t_hsb = [Tr() for _ in range(2)]
            hsT = [sb("hsT%d" % i, [128, 8, MBLK], BF16, p4) for i in range(2)]; t_hsT = [[Tr() for _ in range(MT)] for _ in range(2)]
            sa = [sb("sa%d" % i, [128, 2 * MBLK], F32, p4) for i in range(2)]; t_sa = [Tr() for _ in range(2)]
            hid = [sb("hid%d" % i, [128, 2, MBLK], BF16, p4) for i in range(2)]; t_hid = [Tr() for _ in range(2)]
            yb = [sb("yb%d" % i, [128, D], F32, p4) for i in range(2)]; t_yb = [Tr() for _ in range(2)]
            t_ys = Tr()

            def load_block(b):
                k = b % 2
                if b >= NPRE:
                    load_w(b)
                dma(sp, hsb[k][:], hs.ap()[b * MBLK:(b + 1) * MBLK, :].rearrange("(j p) d -> p j d", p=128),
                    reads=[t_hs, t_hsz], writes=[t_hsb[k]])

            def bT(b):
                k = b % 2
                for j in range(MT):
                    transpose_gain(hsb[k][:, j, :], t_hsb[k], 128, 8, j % 2, hsT[k][:, :, j * 128:(j + 1) * 128], t_hsT[k][j], None, None, interleave=True)

            def bAB(b):
                k = b % 2
                kw = b % NW
                for hc in range(2):
                    for kc in range(8):
                        op(pe, lambda hc=hc, kc=kc: nc.tensor.matmul(PS[2][:, hc * MBLK:(hc + 1) * MBLK], we1[kw][:, kc, hc:256:2], hsT[k][:, kc, :],
                                                                    start=(kc == 0), stop=(kc == 7)),
                           reads=[t_we1[kw]] + t_hsT[k], writes=[TPS[2]], inc=(kc == 7))
                for hc in range(2):
                    for kc in range(8):
                        op(pe, lambda hc=hc, kc=kc: nc.tensor.matmul(PS[3][:, hc * MBLK:(hc + 1) * MBLK], we3[kw][:, kc, hc:256:2], hsT[k][:, kc, :],
                                                                    start=(kc == 0), stop=(kc == 7)),
                           reads=[t_we3[kw]] + t_hsT[k], writes=[TPS[3]], inc=(kc == 7))
                op(act, lambda: nc.scalar.activation(out=sa[k][:, 0:2 * MBLK], in_=PS[2][:, 0:2 * MBLK], func=AF.Silu),
                   reads=[TPS[2]], writes=[t_sa[k]])
                op(dve, lambda: nc.vector.tensor_tensor(out=hid[k][:].rearrange("p c n -> p (c n)"), in0=PS[3][:, 0:2 * MBLK],
                                                        in1=sa[k][:, 0:2 * MBLK], op=ALU.mult),
                   reads=[TPS[3], t_sa[k]], writes=[t_hid[k]])

            def bY(b):
                k = b % 2
                kw = b % NW
                for j in range(MT):
                    yk = (b * MT + j) % 2
                    for hf in range(2):
                        ob = 4 + 2 * (j % 2) + hf
                        for hc in range(2):
                            op(pe, lambda hc=hc, hf=hf, j=j, ob=ob: nc.tensor.matmul(
                                PS[ob][:, :], hid[k][:, hc, j * 128:(j + 1) * 128], we2[kw][:, hc, hf * 512:(hf + 1) * 512],
                                start=(hc == 0), stop=(hc == 1)),
                               reads=[t_hid[k], t_we2[kw]], writes=[TPS[ob]], inc=(hc == 1))
                        if hf == 0:
                            op(act, lambda ob=ob, yk=yk: nc.scalar.copy(out=yb[yk][:, 0:512], in_=PS[ob][:, :]), reads=[TPS[ob]], writes=[t_yb[yk]])
                        else:
                            op(dve, lambda ob=ob, yk=yk: nc.vector.tensor_copy(out=yb[yk][:, 512:1024], in_=PS[ob][:, :]), reads=[TPS[ob]], writes=[t_yb[yk]])
                    dma(sp, ys.ap()[b * MBLK + j * 128:b * MBLK + (j + 1) * 128, :], yb[yk][:], reads=[t_yb[yk]], writes=[t_ys], waw=False)

            if nblk_run > 0:
                load_block(0)
            for step_ in range(nblk_run + 2):
                if step_ + 1 < nblk_run:
                    load_block(step_ + 1)
                if step_ == min(6, nblk_run + 1):
                    wload(wpg, w_pg, 0, 8, D, 0, D, t_wpg)
                    wload(wple, w_ple, 0, 2, D, 0, D, t_wple)
                if step_ < nblk_run:
                    bT(step_)
                if 0 <= step_ - 1 < nblk_run:
                    bAB(step_ - 1)
                if 0 <= step_ - 2 < nblk_run:
                    bY(step_ - 2)

            NXA, NG, NP, NH4, NT4, NS4 = 5, 3, 4, 3, 3, 2
            xa = [sb("xa%d" % i, [128, D], F32, p4) for i in range(NXA)]; t_xa = [Tr() for _ in range(NXA)]
            ga = [sb("ga%d" % i, [128, D], F32, p4) for i in range(NG)]; t_ga = [Tr() for _ in range(NG)]
            gb = [sb("gb%d" % i, [128, D], F32, p4) for i in range(NG)]; t_gb = [Tr() for _ in range(NG)]
            hn4 = [sb("hn4_%d" % i, [128, D], BF16, p4) for i in range(NH4)]; t_hn4 = [Tr() for _ in range(NH4)]
            st4 = [sb("st4_%d" % i, [128, 4], F32, p4) for i in range(NH4)]; t_st4 = [[Tr() for _ in range(4)] for _ in range(NH4)]
            h3T = [sb("h3T%d" % i, [128, 8, 128], BF16, p4) for i in range(NT4)]; t_h3T = [Tr() for _ in range(NT4)]
            pb4 = [sb("pb4_%d" % i, [128, 256], BF16, p4) for i in range(NP)]; t_pb4 = [Tr() for _ in range(NP)]
            pT4 = [sb("pT4_%d" % i, [128, 2, 128], BF16, p4) for i in range(NT4)]; t_pT4 = [Tr() for _ in range(NT4)]
            sg4 = [sb("sg4_%d" % i, [128, D], F32, p4) for i in range(NS4)]; t_sg4 = [Tr() for _ in range(NS4)]
            t_yo = Tr()

            def cA(ti):
                kx, kg, kp = ti % NXA, ti % NG, ti % NP
                dma(sp, xa[kx][:], x1s.ap()[ti * 128:(ti + 1) * 128, :], reads=[t_x1s], writes=[t_xa[kx]])
                dma(pool, ga[kg][:], ys.ap(), reads=[t_ys, t_desti], writes=[t_ga[kg]],
                    indirect=(None, bass.IndirectOffsetOnAxis(ap=desti[:, ti, 0:1], axis=0)))
                dma(pool, gb[kg][:], ys.ap(), reads=[t_ys, t_desti], writes=[t_gb[kg]],
                    indirect=(None, bass.IndirectOffsetOnAxis(ap=desti[:, ti, 1:2], axis=0)))
                dma(pool, pb4[kp][:], pk.ap()[ti * 128:(ti + 1) * 128, :], writes=[t_pb4[kp]])

            def cB(ti):
                kx, kg, kh = ti % NXA, ti % NG, ti % NH4
                op(dve, lambda: nc.vector.scalar_tensor_tensor(out=xa[kx][:], in0=ga[kg][:], scalar=rinfo[:, ti, 4:5], in1=xa[kx][:],
                                                               op0=ALU.mult, op1=ALU.add),
                   reads=[t_ga[kg], t_ri[ti], t_xa[kx]], writes=[t_xa[kx]])
                op(dve, lambda: nc.vector.scalar_tensor_tensor(out=xa[kx][:], in0=gb[kg][:], scalar=rinfo[:, ti, 5:6], in1=xa[kx][:],
                                                               op0=ALU.mult, op1=ALU.add),
                   reads=[t_gb[kg], t_ri[ti], t_xa[kx]], writes=[t_xa[kx]])
                op(act, lambda: nc.scalar.activation(out=hn4[kh][:], in_=xa[kx][:], func=AF.Square, accum_out=st4[kh][:, 0:1]),
                   reads=[t_xa[kx]], writes=[t_hn4[kh], t_st4[kh][0]])
                rstd_from_ss(st4[kh][:, 0:1], st4[kh][:, 2:3], D, t_st4[kh][0], t_st4[kh][2], st4[kh][:, 1:2], t_st4[kh][1])
                op(dve, lambda: nc.vector.tensor_scalar(out=hn4[kh][:], in0=xa[kx][:], scalar1=st4[kh][:, 2:3], scalar2=None, op0=ALU.mult),
                   reads=[t_xa[kx], t_st4[kh][2]], writes=[t_hn4[kh]])

            def cC(ti):
                kh, kt, kp = ti % NH4, ti % NT4, ti % NP
                transpose_gain(hn4[kh], t_hn4[kh], 128, 8, ti % 2, h3T[kt][:], t_h3T[kt], gplec, t_gplec)
                transpose_gain(pb4[kp], t_pb4[kp], 128, 2, 2 + ti % 2, pT4[kt][:], t_pT4[kt], None, None)

            def cD(ti):
                kx, kt, ks = ti % NXA, ti % NT4, ti % NS4
                for hf in range(2):
                    ob = 4 + hf
                    for kc in range(8):
                        op(pe, lambda kc=kc, hf=hf, ob=ob: nc.tensor.matmul(PS[ob][:, :], h3T[kt][:, kc, :], wpg[:, kc, hf * 512:(hf + 1) * 512],
                                                                           start=(kc == 0), stop=(kc == 7)),
                           reads=[t_h3T[kt], t_wpg], writes=[TPS[ob]], inc=(kc == 7))
                    op(act, lambda hf=hf, ob=ob: nc.scalar.activation(out=sg4[ks][:, hf * 512:(hf + 1) * 512], in_=PS[ob][:, :], func=AF.Sigmoid),
                       reads=[TPS[ob]], writes=[t_sg4[ks]])
                    ob2 = 6 + hf
                    for kc in range(2):
                        op(pe, lambda kc=kc, hf=hf, ob2=ob2: nc.tensor.matmul(PS[ob2][:, :], pT4[kt][:, kc, :], wple[:, kc, hf * 512:(hf + 1) * 512],
                                                                             start=(kc == 0), stop=(kc == 1)),
                           reads=[t_pT4[kt], t_wple], writes=[TPS[ob2]], inc=(kc == 1))
                    op(dve, lambda hf=hf, ob2=ob2: nc.vector.tensor_tensor(out=sg4[ks][:, hf * 512:(hf + 1) * 512], in0=PS[ob2][:, :],
                                                                          in1=sg4[ks][:, hf * 512:(hf + 1) * 512], op=ALU.mult),
                       reads=[TPS[ob2], t_sg4[ks]], writes=[t_sg4[ks]])
                op(pool, lambda: nc.gpsimd.tensor_tensor(out=sg4[ks][:], in0=sg4[ks][:], in1=xa[kx][:], op=ALU.add),
                   reads=[t_sg4[ks], t_xa[kx]], writes=[t_sg4[ks]])
                dma(sp, y_o.ap()[ti * 128:(ti + 1) * 128, :], sg4[ks][:], reads=[t_sg4[ks]], writes=[t_yo], waw=False)

            cstages = [cA, cB, cC, cD]
            for step in range(34 + len(cstages) - 1):
                for si, fn in enumerate(cstages):
                    ti = step - si
                    if 0 <= ti < 34:
                        fn(ti)
            Kx.barrier()
            p4.close()

        Kx.barrier()
    return nc


_ROPE_INV = 1.0 / (10000.0 ** (np.arange(0, 32, 2, dtype=np.float32) / 32))


def _rope_tab(pos):
    ang = pos.astype(np.float32)[:, None] * _ROPE_INV[None, :]
    return np.cos(ang).astype(np.float32), np.sin(ang).astype(np.float32)


def make_core_inputs(c, inp):
    b, half = divmod(c, 2)
    own = OWN_GROUPS[half]
    oth = OWN_GROUPS[1 - half]
    order = own + oth
    xp = inp["x_prompt"][b].reshape(16, GRP, D)
    xs = inp["x_sample"][4 * c:4 * c + 4].reshape(256, D)
    xk = np.concatenate([xp[order].reshape(SEQ, D), xs], axis=0)
    pos_p = (np.array(order)[:, None] * GRP + np.arange(GRP)[None, :]).reshape(-1)
    pos_s = np.tile(1024 + np.arange(64), 4)
    pos = np.concatenate([pos_p, pos_s])
    cos, sin = _rope_tab(pos)
    tabk = np.concatenate([cos, cos, -sin, sin], axis=1).astype(np.float32)
    posq = np.concatenate([pos_p[:4096], pos_s])
    cq, sq = _rope_tab(posq)
    tabq = np.concatenate([cq.T, cq.T, -sq.T, sq.T], axis=0).astype(np.float32)
    xh = np.zeros((16, D), np.float32)
    xfull = inp["x_prompt"][b]
    for i, g in enumerate(own):
        if g > 0:
            xh[2 * i:2 * i + 2] = xfull[g * GRP - 2:g * GRP]
    maskb = np.zeros((128, 8), np.float32)
    for i in range(8):
        if N_OTHER_ACT[half][i] < N_OTHER_PROG[i]:
            maskb[:, i] = NEG
    pp = inp["p_prompt"][0, b].reshape(16, GRP, 256)[own].reshape(4096, 256)
    pk = np.concatenate([pp, inp["p_sample"][0, 4 * c:4 * c + 4].reshape(256, 256)], axis=0)
    bq = np.zeros((128, 128), np.float32)
    bq[0:64, 0:64] = 1.0 / 64
    bq[64:96, 64:128] = 1.0 / 32
    cst = np.zeros((128, CSTW), np.float32)
    cst[:, 0:32] = np.arange(32)[None, :]
    cst[:, C_THR:C_THR + NTHR] = (MBLK * np.arange(NTHR))[None, :]
    cst[:, C_BIO:C_BIO + NBLK] = np.arange(NBLK)[None, :]
    cst[:, C_PID] = np.arange(128)
    umat = np.triu(np.ones((128, 128), np.float32), k=1)
    d = {
        "umat": umat, "cst": cst,
        "xk": xk, "xh": xh,
        "latp": inp["cache_kv_latent"][0, 4 * c:4 * c + 4].reshape(4096, 256),
        "krp": inp["cache_k_rope"][0, 4 * c:4 * c + 4].reshape(4096, 32),
        "sconv": inp["state_conv"][0, 4 * c:4 * c + 4],
        "pk": pk, "tabk": tabk, "tabq": tabq, "maskb": maskb,
        "ident": np.eye(128, dtype=np.float32), "bq": bq,
    }
    for k in ("g_mix", "w_in", "g_cq", "w_uq", "g_qn", "g_qr", "g_ckv", "w_ukv", "g_kn", "g_kr", "w_oa",
              "conv_w", "w_oc", "w_o", "g_ffn", "w_rg", "b_rg", "w_re", "b_re", "w1", "w3", "w2",
              "g_ple", "w_pg", "w_ple"):
        d[k] = inp[k][0] if inp[k].ndim >= 3 or k in ("conv_w",) else inp[k]
    return {k: np.ascontiguousarray(v, dtype=np.float32) for k, v in d.items()}


PHASES = (1, 2, 3, 4)
DBG = {}


def kernel(**inputs):
    inp = {k: np.asarray(v) for k, v in inputs.items()}
    nc = build_program(PHASES)
    in_maps = [make_core_inputs(c, inp) for c in range(NCORES)]
    res = run_bass_kernel_spmd(nc, in_maps, core_ids=list(range(NCORES)))
    y_p = np.zeros((4, SEQ, D), np.float32)
    y_s = np.zeros((32, 64, D), np.float32)
    lat_p = np.zeros((1, 4, SEQ, 256), np.float32)
    kpe_p = np.zeros((1, 4, SEQ, 32), np.float32)
    conv_p = np.zeros((1, 4, 2, 512), np.float32)
    lat_s = np.zeros((1, 32, 64, 256), np.float32)
    kpe_s = np.zeros((1, 32, 64, 32), np.float32)
    conv_s = np.zeros((1, 32, 2, 512), np.float32)
    for c in range(NCORES):
        r = res.results[c]
        b, half = divmod(c, 2)
        own = OWN_GROUPS[half]
        for i, g in enumerate(own):
            sl = slice(g * GRP, (g + 1) * GRP)
            y_p[b, sl] = r["y_o"][i * GRP:(i + 1) * GRP]
            lat_p[0, b, sl] = r["lat_o"][i * GRP:(i + 1) * GRP]
            kpe_p[0, b, sl] = r["kpe_o"][i * GRP:(i + 1) * GRP]
        y_s[4 * c:4 * c + 4] = r["y_o"][4096:].reshape(4, 64, D)
        lat_s[0, 4 * c:4 * c + 4] = r["lat_o"][4096:].reshape(4, 64, 256)
        kpe_s[0, 4 * c:4 * c + 4] = r["kpe_o"][4096:].reshape(4, 64, 32)
        conv_s[0, 4 * c:4 * c + 4] = r["conv_o"][1:5]
        if half == 0:
            conv_p[0, b] = r["conv_o"][0]
    return (y_p, y_s, lat_p, kpe_p, conv_p, lat_s, kpe_s, conv_s)
```
